# Optimizing a Trainium2 kernel written in Bass

```python
import math
import jax, jax.numpy as jnp
from jax import lax
import numpy as np

D_MODEL = 1024
BATCH = 8
SEQ = 2048
DEPTH = 1

SB_WIDTH = D_MODEL // 2
SB_HEAD_DIM = 64
SB_HEADS = SB_WIDTH // SB_HEAD_DIM
SB_BLOCK = 128
HGRN_WIDTH = D_MODEL // 2
HGRN_HEAD_DIM = 128
HGRN_HEADS = HGRN_WIDTH // HGRN_HEAD_DIM
HGRN_CHUNK = 64
N_EXPERTS = 32
TOP_K = 4
D_FF = D_MODEL
SWIGLU_ALPHA = 1.702
SWIGLU_LIMIT = 7.0
EXPERT_BLOCK = 128
DEEPNORM_ALPHA = (2 * DEPTH) ** 0.25
DEEPNORM_BETA = (8 * DEPTH) ** -0.25
LN_EPS = 1e-5
RMS_EPS = 1e-6
IN_SPLITS = (SB_WIDTH, SB_WIDTH, SB_WIDTH, HGRN_WIDTH, HGRN_WIDTH, HGRN_WIDTH, HGRN_WIDTH, D_MODEL, D_MODEL)
IN_WIDTH = sum(IN_SPLITS)

kernel_name = 'hybrid_stickbreak_hgrn2_moe_deepnorm'


def layer_norm(x, g, b):
    xf = x.astype(jnp.float32)
    mu = jnp.mean(xf, axis=-1, keepdims=True)
    var = jnp.mean(jnp.square(xf - mu), axis=-1, keepdims=True)
    return ((xf - mu) * lax.rsqrt(var + LN_EPS) * g + b).astype(x.dtype)


def stick_breaking_attention(q, k, v):
    T = q.shape[2]
    scale = SB_HEAD_DIM ** -0.5
    qf, kf, vf = q.astype(jnp.float32), k.astype(jnp.float32), v.astype(jnp.float32)
    outs = []
    for blk in range(T // SB_BLOCK):
        t0 = blk * SB_BLOCK
        t1 = t0 + SB_BLOCK
        z = jnp.einsum('bhtd,bhsd->bhts', qf[:, :, t0:t1], kf[:, :, :t1]) * scale
        mask = jnp.arange(t1)[None, :] < (t0 + jnp.arange(SB_BLOCK))[:, None]
        log_keep = jnp.where(mask, jax.nn.log_sigmoid(-z), 0.0)
        log_after = lax.cumsum(log_keep, axis=3, reverse=True) - log_keep
        weights = jnp.where(mask, jnp.exp(jax.nn.log_sigmoid(z) + log_after), 0.0)
        outs.append(jnp.einsum('bhts,bhsd->bhtd', weights, vf[:, :, :t1]))
    return jnp.concatenate(outs, axis=2).astype(q.dtype)


def hgrn2_recurrence(q, k, logf, v):
    B, T, H, dk = q.shape
    dv = v.shape[-1]
    n_chunks = T // HGRN_CHUNK

    def to_chunks(a):
        return a.reshape(B, n_chunks, HGRN_CHUNK, H, a.shape[-1]).transpose(1, 0, 3, 2, 4)

    causal = jnp.tril(jnp.ones((HGRN_CHUNK, HGRN_CHUNK), dtype=bool))

    def step(S, xs):
        qc, kc, gc, vc = xs
        b = jnp.cumsum(gc, axis=2)
        o_inter = jnp.einsum('bhtk,bhkv->bhtv', qc * jnp.exp(b), S)
        diff = b[:, :, :, None, :] - b[:, :, None, :, :]
        decay = jnp.exp(jnp.where(causal[:, :, None], diff, -jnp.inf))
        scores = jnp.einsum('bhtsk,bhsk->bhts', qc[:, :, :, None, :] * decay, kc)
        o = o_inter + jnp.einsum('bhts,bhsv->bhtv', scores, vc)
        b_last = b[:, :, -1:, :]
        S_new = jnp.exp(b_last[:, :, 0, :])[..., None] * S + jnp.einsum(
            'bhsk,bhsv->bhkv', kc * jnp.exp(b_last - b), vc)
        return S_new, o

    S0 = jnp.zeros((B, H, dk, dv), jnp.float32)
    _, o = lax.scan(step, S0, (to_chunks(q), to_chunks(k), to_chunks(logf), to_chunks(v)))
    return o.transpose(1, 0, 3, 2, 4).reshape(B, T, H, dv)


def hybrid_mixer(x, w_in, lower_bound, hgrn_norm_g, w_branch_sb, w_branch_hgrn, w_out):
    B, T, _ = x.shape
    proj = x @ w_in
    offsets = np.cumsum(IN_SPLITS)[:-1].tolist()
    sq, sk, sv, hq, hf, hi, hg, gate_sb, gate_hg = jnp.split(proj, offsets, axis=-1)

    def heads(a):
        return a.reshape(B, T, SB_HEADS, SB_HEAD_DIM).transpose(0, 2, 1, 3)
    o_sb = stick_breaking_attention(heads(sq), heads(sk), heads(sv))
    o_sb = o_sb.transpose(0, 2, 1, 3).reshape(B, T, SB_WIDTH)

    f = lower_bound + (1.0 - lower_bound) * jax.nn.sigmoid(hf.astype(jnp.float32))
    shp = (B, T, HGRN_HEADS, HGRN_HEAD_DIM)
    o_hg = hgrn2_recurrence(jax.nn.silu(hq.astype(jnp.float32)).reshape(shp),
                            (1.0 - f).reshape(shp),
                            jnp.log(f).reshape(shp),
                            hi.astype(jnp.float32).reshape(shp))
    o_hg = o_hg * lax.rsqrt(jnp.mean(jnp.square(o_hg), axis=-1, keepdims=True) + RMS_EPS)
    o_hg = (o_hg.reshape(B, T, HGRN_WIDTH) * hgrn_norm_g * jax.nn.sigmoid(hg.astype(jnp.float32))).astype(x.dtype)

    merged = jax.nn.sigmoid(gate_sb) * (o_sb @ w_branch_sb) + jax.nn.sigmoid(gate_hg) * (o_hg @ w_branch_hgrn)
    return merged @ w_out


def moe(h, router_w, router_b, w_up, b_up, w_down, b_down):
    B, T, D = h.shape
    N = B * T
    xt = h.reshape(N, D)
    logits = (xt @ router_w + router_b).astype(jnp.float32)
    top_vals, top_idx = lax.top_k(logits, TOP_K)
    gates = jax.nn.softmax(top_vals, axis=-1)

    e_flat = top_idx.reshape(-1)
    tok_flat = jnp.arange(N * TOP_K, dtype=jnp.int32) // TOP_K
    order = jnp.argsort(e_flat)
    e_sorted = e_flat[order]
    tok_sorted = tok_flat[order]
    gate_sorted = gates.reshape(-1)[order].astype(h.dtype)

    counts = jnp.bincount(e_flat, length=N_EXPERTS)
    padded = (counts + EXPERT_BLOCK - 1) // EXPERT_BLOCK * EXPERT_BLOCK
    start = jnp.cumsum(counts) - counts
    pad_end = jnp.cumsum(padded)
    pad_start = pad_end - padded
    dest = pad_start[e_sorted] + (jnp.arange(N * TOP_K) - start[e_sorted])

    cap = N * TOP_K + N_EXPERTS * (EXPERT_BLOCK - 1)
    n_blocks = -(-cap // EXPERT_BLOCK)
    n_slots = n_blocks * EXPERT_BLOCK
    slot_tok = jnp.full((n_slots,), N, dtype=jnp.int32).at[dest].set(tok_sorted)
    x_pad = jnp.concatenate([xt, jnp.zeros((1, D), xt.dtype)], axis=0)
    xb = x_pad[slot_tok].reshape(n_blocks, EXPERT_BLOCK, D)
    block_e = jnp.minimum(jnp.searchsorted(pad_end, jnp.arange(n_blocks) * EXPERT_BLOCK, side='right'),
                          N_EXPERTS - 1)

    def expert_block(args):
        xblk, e = args
        hcat = xblk @ w_up[e] + b_up[e]
        x_glu = jnp.minimum(hcat[:, 0::2], SWIGLU_LIMIT)
        x_lin = jnp.clip(hcat[:, 1::2], -SWIGLU_LIMIT, SWIGLU_LIMIT)
        act = x_glu * jax.nn.sigmoid(SWIGLU_ALPHA * x_glu) * (x_lin + 1.0)
        return act @ w_down[e] + b_down[e]

    y_slots = lax.map(expert_block, (xb, block_e)).reshape(n_slots, D)
    contrib = y_slots[dest] * gate_sorted[:, None]
    out = jax.ops.segment_sum(contrib, tok_sorted, num_segments=N)
    return out.reshape(B, T, D).astype(h.dtype)


def setup_inputs(seed: int = 0) -> dict:
    key = jax.random.key(seed)
    ks = jax.random.split(key, 18)
    nrm = jax.random.normal
    beta = DEEPNORM_BETA
    col_scale = jnp.concatenate([jnp.full((w,), s, jnp.float32) for w, s in
                                 zip(IN_SPLITS, (1.0, 1.0, beta, 1.0, 1.0, beta, 1.0, 1.0, 1.0))])
    x = nrm(ks[0], (BATCH, SEQ, D_MODEL), jnp.float32)
    w_in = nrm(ks[1], (DEPTH, D_MODEL, IN_WIDTH), jnp.float32) * D_MODEL ** -0.5 * col_scale
    hgrn_lb_logits = 1.0 + 0.1 * nrm(ks[2], (DEPTH + 1, HGRN_WIDTH), jnp.float32)
    hgrn_norm_g = 1.0 + 0.05 * nrm(ks[3], (DEPTH, HGRN_WIDTH), jnp.float32)
    w_branch_sb = nrm(ks[4], (DEPTH, SB_WIDTH, D_MODEL), jnp.float32) * SB_WIDTH ** -0.5
    w_branch_hgrn = nrm(ks[5], (DEPTH, HGRN_WIDTH, D_MODEL), jnp.float32) * HGRN_WIDTH ** -0.5
    w_out = nrm(ks[6], (DEPTH, D_MODEL, D_MODEL), jnp.float32) * D_MODEL ** -0.5 * beta
    ln1_g = 1.0 + 0.05 * nrm(ks[7], (DEPTH, D_MODEL), jnp.float32)
    ln1_b = 0.02 * nrm(ks[8], (DEPTH, D_MODEL), jnp.float32)
    router_w = nrm(ks[9], (DEPTH, D_MODEL, N_EXPERTS), jnp.float32) * D_MODEL ** -0.5
    router_b = 0.01 * nrm(ks[10], (DEPTH, N_EXPERTS), jnp.float32)
    expert_w_up = nrm(ks[11], (DEPTH, N_EXPERTS, D_MODEL, 2 * D_FF), jnp.float32) * D_MODEL ** -0.5 * beta
    expert_b_up = 0.01 * nrm(ks[12], (DEPTH, N_EXPERTS, 2 * D_FF), jnp.float32)
    expert_w_down = nrm(ks[13], (DEPTH, N_EXPERTS, D_FF, D_MODEL), jnp.float32) * D_FF ** -0.5 * beta
    expert_b_down = 0.01 * nrm(ks[14], (DEPTH, N_EXPERTS, D_MODEL), jnp.float32)
    ln2_g = 1.0 + 0.05 * nrm(ks[15], (DEPTH, D_MODEL), jnp.float32)
    ln2_b = 0.02 * nrm(ks[16], (DEPTH, D_MODEL), jnp.float32)
    return {'x': x, 'w_in': w_in, 'hgrn_lb_logits': hgrn_lb_logits, 'hgrn_norm_g': hgrn_norm_g,
            'w_branch_sb': w_branch_sb, 'w_branch_hgrn': w_branch_hgrn, 'w_out': w_out,
            'ln1_g': ln1_g, 'ln1_b': ln1_b, 'router_w': router_w, 'router_b': router_b,
            'expert_w_up': expert_w_up, 'expert_b_up': expert_b_up,
            'expert_w_down': expert_w_down, 'expert_b_down': expert_b_down,
            'ln2_g': ln2_g, 'ln2_b': ln2_b}


def reference(x, w_in, hgrn_lb_logits, hgrn_norm_g, w_branch_sb, w_branch_hgrn, w_out,
              ln1_g, ln1_b, router_w, router_b, expert_w_up, expert_b_up,
              expert_w_down, expert_b_down, ln2_g, ln2_b):
    lower_bounds = jnp.cumsum(jax.nn.softmax(hgrn_lb_logits.astype(jnp.float32), axis=0), axis=0)
    h = x
    for l in range(DEPTH):
        mix = hybrid_mixer(h, w_in[l], lower_bounds[l], hgrn_norm_g[l],
                           w_branch_sb[l], w_branch_hgrn[l], w_out[l])
        h = layer_norm(DEEPNORM_ALPHA * h + mix, ln1_g[l], ln1_b[l])
        ffn = moe(h, router_w[l], router_b[l], expert_w_up[l], expert_b_up[l],
                  expert_w_down[l], expert_b_down[l])
        h = layer_norm(DEEPNORM_ALPHA * h + ffn, ln2_g[l], ln2_b[l])
    return h
```

```python
import os
from contextlib import ExitStack

import numpy as np
import concourse.bass as bass
import concourse.mybir as mybir
from concourse.bass_utils import run_bass_kernel_spmd

F32 = mybir.dt.float32
BF16 = mybir.dt.bfloat16
I32 = mybir.dt.int32
U32 = mybir.dt.uint32
AF = mybir.ActivationFunctionType
ALU = mybir.AluOpType
AX = mybir.AxisListType

T = 2048
D = 1024
NE = 32
CAP = 384
NSLOT = NE * CAP
ALPHA = 2.0 ** 0.25
LN_EPS = 1e-5
RMS_EPS = 1e-6
INW = 5632

ENGS = ("pe", "act", "dve", "pool", "sp")


class Sched:
    def __init__(self, nc, stack):
        self.nc = nc
        self.stack = stack
        self.q = {n: [] for n in ENGS}
        self.sems = {}
        self.cnt = {}
        self.waited = {n: {} for n in ENGS}
        self.lastw = {}
        self.readers = {}
        self.regs = {}
        for n in ENGS:
            self._newsem(n)

    def _newsem(self, name):
        self.sems[name] = self.stack.enter_context(self.nc.semaphore("s_" + name))
        self.cnt[name] = 0

    def _deps(self, eng, r, w):
        need = {}

        def add(d):
            for s, v in d.items():
                if eng == "pe" and s == "pe":
                    continue
                if v > need.get(s, 0):
                    need[s] = v
        for k in r:
            add(self.lastw.get(k, {}))
        for k in w:
            add(self.lastw.get(k, {}))
            add(self.readers.get(k, {}))
        waits = []
        wd = self.waited[eng]
        for s, v in need.items():
            if v > wd.get(s, 0):
                wd[s] = v
                waits.append((s, v))
        return waits

    def _register(self, tok, r, w):
        s, v = tok
        for k in w:
            self.lastw[k] = {s: v}
            self.readers[k] = {}
        for k in r:
            d = self.readers.setdefault(k, {})
            if v > d.get(s, 0):
                d[s] = v

    def op(self, eng, fn, r=(), w=()):
        waits = self._deps(eng, r, w)
        self.cnt[eng] += 1
        tok = (eng, self.cnt[eng])
        self.q[eng].append((waits, fn, (eng, 1)))
        self._register(tok, r, w)

    def group(self, eng, fns, r=(), w=()):
        waits = self._deps(eng, r, w)
        self.cnt[eng] += 1
        tok = (eng, self.cnt[eng])
        for i, fn in enumerate(fns):
            self.q[eng].append((waits if i == 0 else [], fn, (eng, 1) if i == len(fns) - 1 else None))
        self._register(tok, r, w)

    def dma(self, queue, fn, chan, r=(), w=()):
        if chan not in self.sems:
            self._newsem(chan)
        waits = self._deps(queue, r, w)
        self.cnt[chan] += 16
        tok = (chan, self.cnt[chan])
        self.q[queue].append((waits, fn, (chan, 16)))
        self._register(tok, r, w)

    def barrier(self, full=False):
        toks = {s: c for s, c in self.cnt.items() if c > 0 and (full or not s.startswith("pc"))}
        for eng in ENGS:
            wd = self.waited[eng]
            waits = []
            for s, v in toks.items():
                if s == eng:
                    continue
                if v > wd.get(s, 0):
                    wd[s] = v
                    waits.append((s, v))
            if waits:
                self.q[eng].append((waits, None, None))

    def emit(self, block):
        sems = self.sems

        def run(e, lst):
            for waits, fn, inc in lst:
                for s, v in waits:
                    e.wait_ge(sems[s], v)
                if fn is not None:
                    ins = fn(e)
                    if inc is not None:
                        ins.then_inc(sems[inc[0]], inc[1])

        block.tensor(lambda e: run(e, self.q["pe"]))
        block.scalar(lambda e: run(e, self.q["act"]))
        block.vector(lambda e: run(e, self.q["dve"]))
        def run_pool(e):
            self.regs["zero"] = e.to_reg(0.0)
            self.regs["bound"] = e.to_reg(NSLOT - 1)
            run(e, self.q["pool"])
        block.gpsimd(run_pool)
        block.sync(lambda e: run(e, self.q["sp"]))


def build_nc(stage=99, dbg=None):
    nc = bass.Bass("TRN2", target_bir_lowering=False)
    dram = lambda n, s, dt=F32: nc.dram_tensor(n, list(s), dt, kind="ExternalInput").ap()
    x = dram("x", [T, D])
    w_in = dram("w_in", [D, INW])
    lbl = dram("hgrn_lb_logits", [2, 512])
    hng = dram("hgrn_norm_g", [1, 512])
    w_bsb = dram("w_branch_sb", [512, D])
    w_bhg = dram("w_branch_hgrn", [512, D])
    w_out = dram("w_out", [D, D])
    ln1g = dram("ln1_g", [1, D]); ln1b = dram("ln1_b", [1, D])
    rw = dram("router_w", [D, NE]); rb = dram("router_b", [1, NE])
    wup = dram("expert_w_up", [NE, D, 2 * D]); bup = dram("expert_b_up", [NE, 2 * D])
    wdn = dram("expert_w_down", [NE, D, D]); bdn = dram("expert_b_down", [NE, D])
    ln2g = dram("ln2_g", [1, D]); ln2b = dram("ln2_b", [1, D])
    out = nc.dram_tensor("out", [T, D], F32, kind="ExternalOutput").ap()
    xg_d = nc.dram_tensor("xg_d", [NSLOT, D], BF16).ap()
    y_d = nc.dram_tensor("y_d", [NSLOT, D], F32).ap()
    h1_d = nc.dram_tensor("h1_d", [T, D], F32).ap()
    PC = [1, 3, 5, 7, 9, 11, 13, 15, 17, 19, 21, 23, 25]
    wb_d = nc.dram_tensor("wb_d", [len(PC), D, 3 * D], BF16).ap()
    dbg_out = {}

    stack = ExitStack()
    with stack:
        S = Sched(nc, stack)
        sb = lambda n, s, dt=F32: stack.enter_context(nc.sbuf_tensor(n, list(s), dt))
        PS = [stack.enter_context(nc.psum_tensor(f"ps{i}", [128, 512], F32)) for i in range(8)]
        PSB = [p.bitcast(BF16) for p in PS]
        pk = lambda i: ("ps", i)

        def dump(name, ap, shape, dt=F32, key=()):
            o = nc.dram_tensor("dbg_" + name, list(shape), dt, kind="ExternalOutput").ap()
            dbg_out[name] = o
            S.dma("sp", lambda e: e.dma_start(out=o, in_=ap), "dbgch", r=key, w=[("dbg", name)])

        identb = sb("identb", [128, 128], BF16)
        identf = sb("identf", [128, 128], F32)
        negUT = sb("negUT", [128, 128], BF16)
        negOnes = sb("negOnes", [128, 128], BF16)
        zerosb = sb("zerosb", [128, 128], BF16)
        LTb = sb("LTb", [128, 128], BF16)
        onesb = sb("onesb", [128, 128], BF16)
        onesDiv = sb("onesDiv", [128, 128], BF16)
        epsln = sb("epsln", [128, 1], F32)
        epsrms = sb("epsrms", [128, 1], F32)
        cvec = sb("cvec", [128, NE], F32)
        lbraw = sb("lbraw", [128, 2, 4], F32)
        lbd = sb("lbd", [128, 4], F32)
        lbT = sb("lbT", [128, 4], F32)
        omlT = sb("omlT", [128, 4], F32)
        nomlT = sb("nomlT", [128, 4], F32)
        gcol = sb("gcol", [128, 4], F32)
        CK = ["consts"]

        def consts(e):
            pass
        S.op("pool", lambda e: e.memset(identb[:], 1.0), w=["c_identb"])
        S.op("pool", lambda e: e.affine_select(out=identb[:], in_=identb[:], pattern=[[1, 128]], compare_op=ALU.is_equal,
                                               fill=S.regs["zero"], base=0, channel_multiplier=-1), r=["c_identb"], w=["c_identb"])
        S.op("pool", lambda e: e.memset(identf[:], 1.0), w=["c_identf"])
        S.op("pool", lambda e: e.affine_select(out=identf[:], in_=identf[:], pattern=[[1, 128]], compare_op=ALU.is_equal,
                                               fill=S.regs["zero"], base=0, channel_multiplier=-1), r=["c_identf"], w=["c_identf"])
        S.op("pool", lambda e: e.memset(negUT[:], -1.0), w=["c_negUT"])
        S.op("pool", lambda e: e.affine_select(out=negUT[:], in_=negUT[:], pattern=[[-1, 128]], compare_op=ALU.is_ge,
                                               fill=S.regs["zero"], base=0, channel_multiplier=1), r=["c_negUT"], w=["c_negUT"])
        S.op("pool", lambda e: e.memset(negOnes[:], -1.0), w=["c_negOnes"])
        S.op("pool", lambda e: e.memset(zerosb[:], 0.0), w=["c_zerosb"])
        S.op("pool", lambda e: e.memset(onesb[:], 1.0), w=["c_onesb"])
        S.op("pool", lambda e: e.memset(onesDiv[:], 1.0 / 128.0), w=["c_onesDiv"])
        S.op("pool", lambda e: e.memset(LTb[:], 1.0), w=["c_LTb"])
        S.op("pool", lambda e: e.affine_select(out=LTb[:], in_=LTb[:], pattern=[[1, 128]], compare_op=ALU.is_gt,
                                               fill=S.regs["zero"], base=0, channel_multiplier=-1), r=["c_LTb"], w=["c_LTb"])
        S.op("pool", lambda e: e.memset(epsln[:], LN_EPS), w=["c_eps"])
        S.op("pool", lambda e: e.memset(epsrms[:], RMS_EPS), w=["c_eps2"])
        for ee in range(NE):
            S.op("pool", lambda e, ee=ee: e.memset(cvec[:, ee:ee + 1], float(NSLOT + 1 - ee * CAP)), w=[("c_cvec", ee)])
        CVK = [("c_cvec", ee) for ee in range(NE)]
        S.dma("sp", lambda e: e.dma_start(out=lbraw[:], in_=lbl.rearrange("r (h p) -> p r h", p=128), allow_slow_non_contiguous=True), "cst0", w=["lbraw"])
        S.dma("sp", lambda e: e.dma_start(out=gcol[:], in_=hng.rearrange("o (h p) -> p (o h)", p=128), allow_slow_non_contiguous=True), "cst1", w=["gcol"])
        S.op("dve", lambda e: e.tensor_tensor(out=lbd[:], in0=lbraw[:, 0, :], in1=lbraw[:, 1, :], op=ALU.subtract), r=["lbraw"], w=["lbd"])
        S.op("act", lambda e: e.activation(out=lbT[:], in_=lbd[:], func=AF.Sigmoid), r=["lbd"], w=["lbT"])
        S.op("dve", lambda e: e.tensor_scalar(out=omlT[:], in0=lbT[:], scalar1=-1.0, scalar2=1.0, op0=ALU.mult, op1=ALU.add), r=["lbT"], w=["omlT"])
        S.op("dve", lambda e: e.tensor_scalar(out=nomlT[:], in0=lbT[:], scalar1=1.0, scalar2=-1.0, op0=ALU.mult, op1=ALU.add), r=["lbT"], w=["nomlT"])

        gk_all = sb("gk_all", [128, 16, 4], F32)
        destk = [[sb(f"dk{tb}_{k}", [128, 1], I32) for k in range(4)] for tb in range(16)]
        stX = ExitStack()
        xT = stX.enter_context(nc.sbuf_tensor("xT", [128, 8, T], BF16))
        stM = ExitStack()
        o_sbT = stM.enter_context(nc.sbuf_tensor("o_sbT", [64, 8, T], BF16))
        o_hgT = stM.enter_context(nc.sbuf_tensor("o_hgT", [128, 4, T], BF16))
        w_in_v = w_in.rearrange("(c p) f -> p c f", p=128)

        XGZ = [("xgz", ee) for ee in range(NE)]
        pc_state = {"n": 0}

        def precast_next():
            n = pc_state["n"]
            if n >= 2 * len(PC) or stage < 7 or pc_state.get("closed"):
                return
            pc_state["n"] = n + 1
            i, which = n // 2, n % 2
            ee = PC[i]
            if which == 0:
                S.dma("pool", lambda e: e.dma_start(out=wb_d[i, :, 0:2 * D], in_=wup[ee]), f"pcu{i % 4}", w=[("pcu", i)])
            else:
                S.dma("pool", lambda e: e.dma_start(out=wb_d[i, :, 2 * D:3 * D], in_=wdn[ee]), f"pcd{i % 4}", w=[("pcd", i)])

        with ExitStack() as st:
            sbl = lambda n, s, dt=F32: st.enter_context(nc.sbuf_tensor(n, list(s), dt))
            xb = sbl("xb", [128, 16, D], BF16)
            xv = x.rearrange("(n p) d -> p n d", p=128)
            for g in range(4):
                S.dma("pool", lambda e, g=g: e.dma_start(out=xb[:, 4 * g:4 * g + 4, :], in_=xv[:, 4 * g:4 * g + 4, :]),
                      f"xb{g}", w=[("xb", g)])
            i = 0
            for tg in range(4):
                for dc in range(8):
                    bank = i % 2
                    S.group("pe", [lambda e, j=j, tg=tg, dc=dc, bank=bank: e.transpose(
                        out=PSB[bank][:, j * 128:(j + 1) * 128], in_=xb[:, tg * 4 + j, dc * 128:(dc + 1) * 128], identity=identb[:])
                        for j in range(4)], r=[("xb", tg), "c_identb"], w=[pk(bank)])
                    eng = "act" if i % 2 == 0 else "dve"
                    if eng == "act":
                        S.op("act", lambda e, tg=tg, dc=dc, bank=bank: e.copy(out=xT[:, dc, tg * 512:(tg + 1) * 512], in_=PSB[bank][:, 0:512]),
                             r=[pk(bank)], w=[("xT", tg)])
                    else:
                        S.op("dve", lambda e, tg=tg, dc=dc, bank=bank: e.tensor_copy(out=xT[:, dc, tg * 512:(tg + 1) * 512], in_=PSB[bank][:, 0:512]),
                             r=[pk(bank)], w=[("xT", tg)])
                    i += 1
            S.barrier()
        XTK = [("xT", tg) for tg in range(4)]
        if stage == 1:
            dump("xT", xT[:], [128, 8, T], BF16, key=XTK)

        if stage >= 2:
          with ExitStack() as st:
            sbl = lambda n, s, dt=F32: st.enter_context(nc.sbuf_tensor(n, list(s), dt))
            zrow = sbl("zrow", [128, 3, D], BF16)
            S.op("pool", lambda e: e.memset(zrow[:], 0.0), w=["zrow"])
            for ee in range(NE):
                S.dma("sp", lambda e, ee=ee: e.dma_start(out=xg_d[ee * CAP:(ee + 1) * CAP, :].rearrange("(s p) d -> p s d", p=128), in_=zrow[:]),
                      "xgz", r=["zrow"], w=[("xgz", ee)])
            QTs = sbl("QTs", [128, 4, T], BF16)
            KT = sbl("KT", [128, 4, T], BF16)
            V = sbl("V", [128, 16, 512], BF16)
            wg = [sbl(f"wg{i}", [128, 8, 512], BF16) for i in range(2)]
            for g in range(3):
                sl = g % 2
                S.dma("pool", lambda e, g=g, sl=sl: e.dma_start(out=wg[sl][:], in_=w_in_v[:, :, g * 512:(g + 1) * 512]),
                      f"wg{sl}", w=[("wg", sl)])
                if g < 2:
                    dst = QTs if g == 0 else KT
                    i = 0
                    for fb in range(4):
                        for tc in range(4):
                            bank = 2 + (i % 2)
                            S.group("pe", [lambda e, dc=dc, fb=fb, tc=tc, bank=bank, sl=sl: e.matmul(
                                PS[bank][:, :], lhsT=wg[sl][:, dc, fb * 128:(fb + 1) * 128], rhs=xT[:, dc, tc * 512:(tc + 1) * 512],
                                start=(dc == 0), stop=(dc == 7)) for dc in range(8)], r=[("wg", sl), ("xT", tc)], w=[pk(bank)])
                            sc = 0.125 if g == 0 else 1.0
                            if i % 2 == 0:
                                S.op("act", lambda e, dst=dst, fb=fb, tc=tc, bank=bank, sc=sc: e.activation(
                                    out=dst[:, fb, tc * 512:(tc + 1) * 512], in_=PS[bank][:, :], func=AF.Copy, scale=sc),
                                    r=[pk(bank)], w=[("qk", g, fb)])
                            else:
                                S.op("dve", lambda e, dst=dst, fb=fb, tc=tc, bank=bank, sc=sc: e.tensor_scalar(
                                    out=dst[:, fb, tc * 512:(tc + 1) * 512], in0=PS[bank][:, :], scalar1=sc, scalar2=None, op0=ALU.mult),
                                    r=[pk(bank)], w=[("qk", g, fb)])
                            i += 1
                else:
                    for tb in range(16):
                        bank = 2 + (tb % 2)
                        S.group("pe", [lambda e, dc=dc, tb=tb, bank=bank, sl=sl: e.matmul(
                            PS[bank][:, :], lhsT=xT[:, dc, tb * 128:(tb + 1) * 128], rhs=wg[sl][:, dc, :],
                            start=(dc == 0), stop=(dc == 7)) for dc in range(8)], r=[("wg", sl), ("xT", tb // 4)], w=[pk(bank)])
                        if tb % 2 == 0:
                            S.op("act", lambda e, tb=tb, bank=bank: e.copy(out=V[:, tb, :], in_=PS[bank][:, :]), r=[pk(bank)], w=[("V", tb)])
                        else:
                            S.op("dve", lambda e, tb=tb, bank=bank: e.tensor_copy(out=V[:, tb, :], in_=PS[bank][:, :]), r=[pk(bank)], w=[("V", tb)])
            if stage == 2:
                dump("QTs", QTs[:], [128, 4, T], BF16, key=[("qk", 0, fb) for fb in range(4)])
                dump("V", V[:], [128, 16, 512], BF16, key=[("V", tb) for tb in range(16)])

            if stage >= 3:
                Lsum32 = sbl("Lsum32", [128, T], F32)
                Lsum16 = sbl("Lsum16", [128, T], BF16)
                NB = 5
                Eb = [sbl(f"Eb{i}", [128, 512], F32) for i in range(NB)]
                SPb = [sbl(f"SPb{i}", [128, 512], BF16) for i in range(NB)]
                WTb = [sbl(f"WTb{i}", [128, 512], BF16) for i in range(NB)]
                units = []
                for h in range(8):
                    hu = []
                    for kb in range(15, -1, -1):
                        kt0 = kb * 128
                        j0 = kt0 // 512
                        for j in range(j0, 4):
                            t0 = max(kt0, j * 512)
                            hu.append(dict(h=h, kb=kb, j=j, t0=t0, w=(j + 1) * 512 - t0, first=(j == j0), off=t0 - j * 512,
                                           kt0=kt0, hfirst=False, hlast=False))
                    hu[0]["hfirst"] = True
                    hu[-1]["hlast"] = True
                    units += hu
                for i, un in enumerate(units):
                    un["bank"] = i % 4
                    un["sl"] = i % NB

                def st_a(un):
                    h, w, t0, kt0, bank, sl = un["h"], un["w"], un["t0"], un["kt0"], un["bank"], un["sl"]
                    pr, p0 = h // 2, (h % 2) * 64
                    zt = PS[bank]
                    S.op("pe", lambda e: e.matmul(zt[:, 0:w], lhsT=KT[p0:p0 + 64, pr, kt0:kt0 + 128], rhs=QTs[p0:p0 + 64, pr, t0:t0 + w],
                                                  start=True, stop=True), r=[("qk", 0, pr), ("qk", 1, pr)], w=[pk(bank)])
                    S.op("act", lambda e: e.activation(out=Eb[sl][:, 0:w], in_=zt[:, 0:w], func=AF.Exp), r=[pk(bank)], w=[("E", sl)])

                def st_b(un):
                    w, sl = un["w"], un["sl"]
                    S.op("act", lambda e: e.activation(out=SPb[sl][:, 0:w], in_=Eb[sl][:, 0:w], func=AF.Ln, bias=1.0),
                         r=[("E", sl)], w=[("SP", sl)])
                    if un["first"]:
                        S.op("pool", lambda e: e.affine_select(
                            out=SPb[sl][:, 0:128], in_=SPb[sl][:, 0:128], pattern=[[1, 128]], compare_op=ALU.is_gt,
                            fill=S.regs["zero"], base=0, channel_multiplier=-1), r=[("SP", sl)], w=[("SP", sl)])

                def st_c(un):
                    h, kb, j, w, t0, off, bank, sl = un["h"], un["kb"], un["j"], un["w"], un["t0"], un["off"], un["bank"], un["sl"]
                    zt = PS[bank]
                    if un["hfirst"]:
                        S.op("pool", lambda e: e.memset(Lsum32[:], 0.0), w=[("L32", jj) for jj in range(4)])
                        S.op("pool", lambda e: e.memset(Lsum16[:], 0.0), w=[("L16", jj) for jj in range(4)])
                    fns = [lambda e: e.matmul(zt[:, 0:w], lhsT=negUT[:, :], rhs=SPb[sl][:, 0:w], start=False, stop=(kb == 15), skip_group_check=True)]
                    rd = [("SP", sl), "c_negUT"]
                    if kb < 15:
                        fns.append(lambda e: e.matmul(zt[:, 0:w], lhsT=negOnes[:, :], rhs=Lsum16[:, t0:t0 + w], start=False, stop=True, skip_group_check=True))
                        rd += [("L16", j), "c_negOnes"]
                    S.group("pe", fns, r=rd, w=[pk(bank)])
                    S.op("act", lambda e: e.activation(out=WTb[sl][:, 0:w], in_=zt[:, 0:w], func=AF.Exp), r=[pk(bank)], w=[("WT", sl)])
                    if un["first"]:
                        S.op("pool", lambda e: e.affine_select(
                            out=WTb[sl][:, 0:128], in_=WTb[sl][:, 0:128], pattern=[[1, 128]], compare_op=ALU.is_gt,
                            fill=S.regs["zero"], base=0, channel_multiplier=-1), r=[("WT", sl)], w=[("WT", sl)])
                    if kb > 0:
                        S.op("dve", lambda e: e.tensor_tensor(out=Lsum32[:, t0:t0 + w], in0=Lsum32[:, t0:t0 + w], in1=SPb[sl][:, 0:w], op=ALU.add),
                             r=[("SP", sl), ("L32", j)], w=[("L32", j)])
                        S.op("dve", lambda e: e.tensor_copy(out=Lsum16[:, t0:t0 + w], in_=Lsum32[:, t0:t0 + w]), r=[("L32", j)], w=[("L16", j)])

                def st_d(un):
                    h, kb, j, w, t0, off, bank, sl = un["h"], un["kb"], un["j"], un["w"], un["t0"], un["off"], un["bank"], un["sl"]
                    if un["hfirst"]:
                        for jj in range(4):
                            S.op("pe", lambda e, jj=jj: e.matmul(PS[4 + jj][0:64, :], lhsT=zerosb[:, 0:64], rhs=Lsum16[:, jj * 512:(jj + 1) * 512],
                                                               start=True, stop=False, skip_group_check=True),
                                 r=["c_zerosb", ("L16", jj)], w=[pk(4 + jj)])
                    S.op("pe", lambda e: e.matmul(PS[4 + j][0:64, off:off + w], lhsT=V[:, kb, h * 64:(h + 1) * 64], rhs=WTb[sl][:, 0:w],
                                                  start=False, stop=(kb == 0), skip_group_check=True), r=[("WT", sl), ("V", kb)], w=[pk(4 + j)])
                    if un["hlast"]:
                        for jj in range(4):
                            if jj % 2 == 0:
                                S.op("act", lambda e, jj=jj: e.copy(out=o_sbT[:, h, jj * 512:(jj + 1) * 512], in_=PS[4 + jj][0:64, :]),
                                     r=[pk(4 + jj)], w=[("o_sbT", h)])
                            else:
                                S.op("dve", lambda e, jj=jj: e.tensor_copy(out=o_sbT[:, h, jj * 512:(jj + 1) * 512], in_=PS[4 + jj][0:64, :]),
                                     r=[pk(4 + jj)], w=[("o_sbT", h)])

                stages_ = [st_a, st_b, st_c, st_d]
                n = len(units)
                for step in range(n + len(stages_) - 1):
                    for k_, f_ in enumerate(stages_):
                        idx = step - k_
                        if 0 <= idx < n:
                            f_(units[idx])
                    if step % 44 in (4, 32) and step < n - 20:
                        precast_next()
            S.barrier()
        OSK = [("o_sbT", h) for h in range(8)]
        if stage == 3:
            dump("o_sbT", o_sbT[:], [64, 8, T], BF16, key=OSK)

        if stage >= 4:
          with ExitStack() as st:
            sbl = lambda n, s, dt=F32: st.enter_context(nc.sbuf_tensor(n, list(s), dt))
            maskLE8 = sbl("maskLE8", [64, 8, 64], F32)
            rmask = sbl("rmask", [128, 32, 64], F32)
            S.op("pool", lambda e: e.memset(maskLE8[:], 1.0), w=["c_maskLE8"])
            S.op("pool", lambda e: e.affine_select(out=maskLE8[:], in_=maskLE8[:], pattern=[[0, 8], [1, 64]], compare_op=ALU.is_ge,
                                                   fill=S.regs["zero"], base=0, channel_multiplier=-1), r=["c_maskLE8"], w=["c_maskLE8"])
            S.op("pool", lambda e: e.memset(rmask[:], 1.0), w=["c_rmask"])
            S.op("pool", lambda e: e.affine_select(out=rmask[:], in_=rmask[:], pattern=[[0, 32], [1, 64]], compare_op=ALU.is_gt,
                                                   fill=S.regs["zero"], base=0, channel_multiplier=0), r=["c_rmask"], w=["c_rmask"])
            wh = [sbl(f"wh{i}", [128, 8, 4, 128], BF16) for i in range(2)]
            sgq = [sbl(f"sgq{i}", [128, 512], F32) for i in range(2)]
            qT = sbl("qT", [128, T], F32)
            kT = sbl("kT", [128, T], F32)
            fT = sbl("fT", [128, T], F32)
            gT = fT
            bT = sbl("bT", [128, T], F32)
            ebt = sbl("ebt", [128, T], F32)
            sgT = sbl("sgT", [128, T], BF16)
            qeT = sbl("qeT", [128, T], BF16)
            keT = sbl("keT", [128, T], BF16)
            klT = sbl("klT", [128, T], BF16)
            elast = sbl("elast", [128, 32], F32)
            vtok = sbl("vtok", [64, 32, 128], BF16)
            kltok = sbl("kltok", [64, 32, 128], BF16)
            scT = [sbl(f"scT{i}", [64, 8, 64], BF16) for i in range(2)]
            S32 = [sbl(f"S32_{i}", [128, 128], F32) for i in range(2)]
            S16 = [sbl(f"S16_{i}", [128, 128], BF16) for i in range(4)]
            oT = qT
            osq = klT
            rs = [sbl(f"rs{i}", [128, 512], F32) for i in range(2)]
            onr = [sbl(f"onr{i}", [128, 512], F32) for i in range(2)]
            def load_wh(hh):
                sl = hh % 2
                for kk in range(4):
                    c0 = 1536 + kk * 512 + hh * 128
                    S.dma("pool", lambda e, sl=sl, kk=kk, c0=c0: e.dma_start(out=wh[sl][:, :, kk, :], in_=w_in_v[:, :, c0:c0 + 128]),
                          f"wh{sl}_{kk}", w=[("wh", sl, kk)])
            load_wh(0)
            for hh in range(4):
                sl = hh % 2
                if hh + 1 < 4:
                    load_wh(hh + 1)
                precast_next()
                precast_next()
                i = 0
                for kk in (0, 1, 3):
                    for tc in range(4):
                        bank = i % 2
                        cs = slice(tc * 512, (tc + 1) * 512)
                        S.group("pe", [lambda e, dc=dc, kk=kk, tc=tc, bank=bank, sl=sl: e.matmul(
                            PS[bank][:, :], lhsT=wh[sl][:, dc, kk, :], rhs=xT[:, dc, tc * 512:(tc + 1) * 512],
                            start=(dc == 0), stop=(dc == 7)) for dc in range(8)], r=[("wh", sl, kk), ("xT", tc)], w=[pk(bank)])
                        sq_ = sgq[i % 2]
                        if kk == 0:
                            S.op("act", lambda e, bank=bank, sq_=sq_: e.activation(out=sq_[:], in_=PS[bank][:, :], func=AF.Sigmoid),
                                 r=[pk(bank)], w=[("sgq", i % 2)])
                            S.op("dve", lambda e, bank=bank, sq_=sq_, cs=cs: e.tensor_tensor(out=qT[:, cs], in0=PS[bank][:, :], in1=sq_[:], op=ALU.mult),
                                 r=[pk(bank), ("sgq", i % 2)], w=["qT"])
                        elif kk == 1:
                            S.op("act", lambda e, bank=bank, sq_=sq_: e.activation(out=sq_[:], in_=PS[bank][:, :], func=AF.Sigmoid),
                                 r=[pk(bank)], w=[("sgq", i % 2)])
                            S.op("dve", lambda e, sq_=sq_, cs=cs, hh=hh: e.tensor_scalar(
                                out=fT[:, cs], in0=sq_[:], scalar1=omlT[:, hh:hh + 1], scalar2=lbT[:, hh:hh + 1], op0=ALU.mult, op1=ALU.add),
                                r=[("sgq", i % 2), "omlT", "lbT"], w=["fT"])
                            S.op("dve", lambda e, sq_=sq_, cs=cs, hh=hh: e.tensor_scalar(
                                out=kT[:, cs], in0=sq_[:], scalar1=nomlT[:, hh:hh + 1], scalar2=omlT[:, hh:hh + 1], op0=ALU.mult, op1=ALU.add),
                                r=[("sgq", i % 2), "omlT", "nomlT"], w=["kT"])
                        else:
                            S.op("act", lambda e, bank=bank, cs=cs: e.activation(out=sgT[:, cs], in_=PS[bank][:, :], func=AF.Sigmoid),
                                 r=[pk(bank)], w=["sgT"])
                        i += 1
                for g4 in range(8):
                    bank = 2 + g4 % 2
                    fns = []
                    for jj in range(4):
                        c = g4 * 4 + jj
                        for dc in range(8):
                            fns.append(lambda e, dc=dc, c=c, jj=jj, bank=bank, sl=sl: e.matmul(
                                PS[bank][0:64, jj * 128:(jj + 1) * 128], lhsT=xT[:, dc, c * 64:(c + 1) * 64], rhs=wh[sl][:, dc, 2, :],
                                start=(dc == 0), stop=(dc == 7), skip_group_check=True))
                    S.group("pe", fns, r=[("wh", sl, 2)] + XTK, w=[pk(bank)])
                    S.op("act", lambda e, g4=g4, bank=bank: e.copy(out=vtok[:, g4 * 4:(g4 + 1) * 4, :], in_=PS[bank][0:64, :].rearrange("p (a b) -> p a b", b=128)),
                         r=[pk(bank)], w=["vtok"])
                S.op("act", lambda e: e.activation(out=gT[:], in_=fT[:], func=AF.Ln), r=["fT"], w=["fT"])
                S.op("dve", lambda e: e.tensor_tensor_scan(out=bT[:], data0=rmask[:].rearrange("p a b -> p (a b)"), data1=gT[:], initial=0.0,
                                                           op0=ALU.mult, op1=ALU.add), r=["fT", "c_rmask"], w=["bT"])
                S.op("act", lambda e: e.activation(out=elast[:], in_=bT[:, 63::64], func=AF.Exp), r=["bT"], w=["elast"])
                S.op("act", lambda e: e.activation(out=ebt[:], in_=bT[:], func=AF.Exp), r=["bT"], w=["ebt"])
                S.op("dve", lambda e: e.tensor_tensor(out=qeT[:], in0=qT[:], in1=ebt[:], op=ALU.mult), r=["qT", "ebt"], w=["qeT"])
                S.op("act", lambda e: e.activation(out=ebt[:], in_=bT[:], func=AF.Exp, scale=-1.0), r=["bT"], w=["ebt"])
                S.op("dve", lambda e: e.tensor_tensor(out=keT[:], in0=kT[:], in1=ebt[:], op=ALU.mult), r=["kT", "ebt"], w=["keT"])
                for c in range(32):
                    S.op("act", lambda e, c=c: e.activation(out=ebt[:, c * 64:(c + 1) * 64], in_=bT[:, c * 64:(c + 1) * 64], func=AF.Exp,
                                                            scale=-1.0, bias=bT[:, c * 64 + 63:c * 64 + 64]), r=["bT"], w=["ebt"])
                S.op("dve", lambda e: e.tensor_tensor(out=klT[:], in0=kT[:], in1=ebt[:], op=ALU.mult), r=["kT", "ebt"], w=["klT"])
                for g8 in range(4):
                    bank = 2 + g8 % 2
                    S.group("pe", [lambda e, jj=jj, g8=g8, bank=bank: e.transpose(
                        out=PSB[bank][0:64, jj * 128:(jj + 1) * 128], in_=klT[:, (g8 * 8 + jj) * 64:(g8 * 8 + jj + 1) * 64], identity=identb[:])
                        for jj in range(8)], r=["klT", "c_identb"], w=[pk(bank)])
                    S.op("dve", lambda e, g8=g8, bank=bank: e.tensor_copy(
                        out=kltok[:, g8 * 8:(g8 + 1) * 8, :], in_=PSB[bank][0:64, :].rearrange("p (a b) -> p a b", b=128)),
                        r=[pk(bank)], w=["kltok"])
                for g8 in range(4):
                    if g8 == 2:
                        precast_next()
                    scb = 4 + g8 % 2
                    ssl = g8 % 2
                    S.group("pe", [lambda e, jj=jj, g8=g8, scb=scb: e.matmul(
                        PS[scb][0:64, jj * 64:(jj + 1) * 64], lhsT=keT[:, (g8 * 8 + jj) * 64:(g8 * 8 + jj + 1) * 64],
                        rhs=qeT[:, (g8 * 8 + jj) * 64:(g8 * 8 + jj + 1) * 64], start=True, stop=True, skip_group_check=True)
                        for jj in range(8)], r=["keT", "qeT"], w=[pk(scb)])
                    S.op("dve", lambda e, scb=scb, ssl=ssl: e.tensor_tensor(
                        out=scT[ssl][:], in0=PS[scb][0:64, :].rearrange("p (a b) -> p a b", b=64), in1=maskLE8[:], op=ALU.mult),
                        r=[pk(scb), "c_maskLE8"], w=[("scT", ssl)])
                    ob = 6 + g8 % 2
                    for jj in range(8):
                        c = g8 * 8 + jj
                        if jj % 4 == 0:
                            db = 2 + (c // 4) % 2
                            S.group("pe", [lambda e, q=q, c=c, db=db: e.matmul(
                                PS[db][:, q * 128:(q + 1) * 128], lhsT=kltok[:, c + q, :], rhs=vtok[:, c + q, :],
                                start=True, stop=True, skip_group_check=True) for q in range(4)], r=["kltok", "vtok"], w=[pk(db)])
                        db = 2 + (c // 4) % 2
                        cur = c % 4
                        fns = []
                        rd = [("scT", ssl), "vtok", "qeT"]
                        if c > 0:
                            fns.append(lambda e, ob=ob, jj=jj, c=c, cur=cur: e.matmul(
                                PS[ob][:, jj * 64:(jj + 1) * 64], lhsT=S16[cur][:, :], rhs=qeT[:, c * 64:(c + 1) * 64],
                                start=True, stop=False, skip_group_check=True))
                            rd.append(("S16", cur))
                        fns.append(lambda e, ob=ob, jj=jj, c=c, ssl=ssl: e.matmul(
                            PS[ob][:, jj * 64:(jj + 1) * 64], lhsT=vtok[:, c, :], rhs=scT[ssl][:, jj, :],
                            start=(c == 0), stop=True, skip_group_check=True))
                        S.group("pe", fns, r=rd, w=[pk(ob)])
                        nxt = (c + 1) % 4
                        so, sn = c % 2, (c + 1) % 2
                        dsl = PS[db][:, (c % 4) * 128:(c % 4 + 1) * 128]
                        if c == 0:
                            S.op("dve", lambda e, dsl=dsl, sn=sn: e.tensor_copy(out=S32[sn][:], in_=dsl), r=[pk(db)], w=[("S32", sn)])
                            S.op("act", lambda e, nxt=nxt, sn=sn: e.copy(out=S16[nxt][:], in_=S32[sn][:]), r=[("S32", sn)], w=[("S16", nxt)])
                        elif c < 31:
                            S.op("dve", lambda e, dsl=dsl, c=c, so=so, sn=sn: e.scalar_tensor_tensor(
                                out=S32[sn][:], in0=S32[so][:], scalar=elast[:, c:c + 1], in1=dsl, op0=ALU.mult, op1=ALU.add),
                                r=[pk(db), ("S32", so), "elast"], w=[("S32", sn)])
                            S.op("act", lambda e, nxt=nxt, sn=sn: e.copy(out=S16[nxt][:], in_=S32[sn][:]), r=[("S32", sn)], w=[("S16", nxt)])
                    S.op("act", lambda e, ob=ob, g8=g8: e.copy(out=oT[:, g8 * 512:(g8 + 1) * 512], in_=PS[ob][:, :]), r=[pk(ob)], w=["qT"])
                S.op("act", lambda e: e.activation(out=osq[:], in_=oT[:], func=AF.Square), r=["qT"], w=["klT"])
                for tc in range(4):
                    bank = tc % 2
                    cs = slice(tc * 512, (tc + 1) * 512)
                    S.op("pe", lambda e, bank=bank, cs=cs: e.matmul(PS[bank][:, :], lhsT=onesDiv[:, :], rhs=osq[:, cs], start=True, stop=True),
                         r=["klT", "c_onesDiv"], w=[pk(bank)])
                    S.op("act", lambda e, bank=bank, tc=tc: e.activation(out=rs[tc % 2][:], in_=PS[bank][:, :], func=AF.Sqrt, bias=epsrms[:, 0:1]),
                         r=[pk(bank), "c_eps2"], w=[("rs", tc % 2)])
                    S.op("dve", lambda e, tc=tc: e.reciprocal(out=rs[tc % 2][:], in_=rs[tc % 2][:]), r=[("rs", tc % 2)], w=[("rs", tc % 2)])
                    S.op("dve", lambda e, tc=tc, cs=cs, hh=hh: e.scalar_tensor_tensor(
                        out=onr[tc % 2][:], in0=oT[:, cs], scalar=gcol[:, hh:hh + 1], in1=rs[tc % 2][:], op0=ALU.mult, op1=ALU.mult),
                        r=["qT", ("rs", tc % 2), "gcol"], w=[("onr", tc % 2)])
                    S.op("dve", lambda e, tc=tc, cs=cs, hh=hh: e.tensor_tensor(out=o_hgT[:, hh, cs], in0=onr[tc % 2][:], in1=sgT[:, cs], op=ALU.mult),
                         r=[("onr", tc % 2), "sgT"], w=[("o_hgT", hh)])
            S.barrier()
        OHK = [("o_hgT", hh) for hh in range(4)]
        for c_ in list(S.cnt):
            if c_.startswith("pc") and S.cnt[c_] > 0:
                S.lastw[("pcall", c_)] = {c_: S.cnt[c_]}
        pc_state["closed"] = True
        if stage == 4:
            dump("o_hgT", o_hgT[:], [128, 4, T], BF16, key=OHK)


        def layer_norm(sl, src, dst, Gt, Bt, tag, stats, mv, rstd, rsl=None):
            rsl = sl if rsl is None else rsl
            for hf_ in range(2):
                S.op("dve", lambda e, hf_=hf_: e.bn_stats(out=stats[sl][:, hf_, :], in_=src[:, hf_ * 512:(hf_ + 1) * 512]),
                     r=[(tag + "r", rsl)], w=[(tag + "st", sl, hf_)])
            S.op("dve", lambda e: e.bn_aggr(out=mv[sl][:], in_=stats[sl][:].rearrange("p a b -> p (a b)")),
                 r=[(tag + "st", sl, 0), (tag + "st", sl, 1)], w=[(tag + "mv", sl)])
            S.op("act", lambda e: e.activation(out=rstd[sl][:], in_=mv[sl][:, 1:2], func=AF.Sqrt, bias=epsln[:, 0:1]),
                 r=[(tag + "mv", sl), "c_eps"], w=[(tag + "rstd", sl)])
            S.op("dve", lambda e: e.reciprocal(out=rstd[sl][:], in_=rstd[sl][:]), r=[(tag + "rstd", sl)], w=[(tag + "rstd", sl)])
            S.op("dve", lambda e: e.tensor_scalar(out=src[:], in0=src[:], scalar1=mv[sl][:, 0:1], scalar2=rstd[sl][:, 0:1],
                                                  op0=ALU.subtract, op1=ALU.mult),
                 r=[(tag + "r", rsl), (tag + "mv", sl), (tag + "rstd", sl)], w=[(tag + "r", rsl)])
            S.op("pool", lambda e: e.tensor_tensor(out=dst[:], in0=src[:], in1=Gt[:], op=ALU.mult),
                 r=[(tag + "r", rsl), tag + "G"], w=[(tag + "o", sl)])
            S.op("pool", lambda e: e.tensor_tensor(out=dst[:], in0=dst[:], in1=Bt[:], op=ALU.add),
                 r=[(tag + "o", sl), tag + "B"], w=[(tag + "o", sl)])

        if stage >= 5:
          with ExitStack() as st:
            sbl = lambda n, s, dt=F32: st.enter_context(nc.sbuf_tensor(n, list(s), dt))
            wgate = sbl("wgate", [128, 8, 2, D], BF16)
            wbsb = sbl("wbsb", [64, 8, D], BF16)
            wbhg = sbl("wbhg", [128, 4, D], BF16)
            mstage = sbl("mstage", [128, 8, 512], BF16)
            sg1 = [sbl(f"sg1_{i}", [128, 512], F32) for i in range(2)]
            sg2 = [sbl(f"sg2_{i}", [128, 512], F32) for i in range(2)]
            S.dma("pool", lambda e: e.dma_start(out=wbsb[:], in_=w_bsb.rearrange("(h p) f -> p h f", p=64)), "wbsb", w=["wbsb"])
            S.dma("pool", lambda e: e.dma_start(out=wbhg[:], in_=w_bhg.rearrange("(h p) f -> p h f", p=128)), "wbhg", w=["wbhg"])
            for q4 in range(4):
                for kk in range(2):
                    c0 = 3584 + kk * 1024 + q4 * 256
                    S.dma("pool", lambda e, kk=kk, c0=c0, q4=q4: e.dma_start(out=wgate[:, :, kk, q4 * 256:(q4 + 1) * 256], in_=w_in_v[:, :, c0:c0 + 256]),
                          f"wgate{q4}_{kk}", w=[("wgate", q4, kk)])
            i = 0
            for tc in range(4):
                cs = slice(tc * 512, (tc + 1) * 512)
                for fb in range(8):
                    sl = i % 2
                    b0 = 4 * (i % 2)
                    for kk in range(2):
                        S.group("pe", [lambda e, dc=dc, kk=kk, fb=fb, b0=b0, cs=cs: e.matmul(
                            PS[b0 + kk][:, :], lhsT=wgate[:, dc, kk, fb * 128:(fb + 1) * 128], rhs=xT[:, dc, cs], start=(dc == 0), stop=(dc == 7))
                            for dc in range(8)], r=[("wgate", fb // 2, kk), ("xT", tc)], w=[pk(b0 + kk)])
                    S.group("pe", [lambda e, h=h, fb=fb, b0=b0, cs=cs: e.matmul(
                        PS[b0 + 2][:, :], lhsT=wbsb[:, h, fb * 128:(fb + 1) * 128], rhs=o_sbT[:, h, cs], start=(h == 0), stop=(h == 7))
                        for h in range(8)], r=["wbsb"] + OSK, w=[pk(b0 + 2)])
                    S.group("pe", [lambda e, hh=hh, fb=fb, b0=b0, cs=cs: e.matmul(
                        PS[b0 + 3][:, :], lhsT=wbhg[:, hh, fb * 128:(fb + 1) * 128], rhs=o_hgT[:, hh, cs], start=(hh == 0), stop=(hh == 3))
                        for hh in range(4)], r=["wbhg"] + OHK, w=[pk(b0 + 3)])
                    S.op("act", lambda e, sl=sl, b0=b0: e.activation(out=sg1[sl][:], in_=PS[b0][:, :], func=AF.Sigmoid), r=[pk(b0)], w=[("sg1", sl)])
                    S.op("act", lambda e, sl=sl, b0=b0: e.activation(out=sg2[sl][:], in_=PS[b0 + 1][:, :], func=AF.Sigmoid), r=[pk(b0 + 1)], w=[("sg2", sl)])
                    S.op("dve", lambda e, sl=sl, b0=b0: e.tensor_tensor(out=sg1[sl][:], in0=sg1[sl][:], in1=PS[b0 + 2][:, :], op=ALU.mult),
                         r=[("sg1", sl), pk(b0 + 2)], w=[("sg1", sl)])
                    S.op("dve", lambda e, sl=sl, b0=b0: e.tensor_tensor(out=sg2[sl][:], in0=sg2[sl][:], in1=PS[b0 + 3][:, :], op=ALU.mult),
                         r=[("sg2", sl), pk(b0 + 3)], w=[("sg2", sl)])
                    S.op("pool", lambda e, sl=sl, fb=fb: e.tensor_tensor(out=mstage[:, fb, :], in0=sg1[sl][:], in1=sg2[sl][:], op=ALU.add),
                         r=[("sg1", sl), ("sg2", sl)], w=[("mstage", fb)])
                    i += 1
                S.op("act", lambda e, cs=cs: e.copy(out=xT[:, :, cs], in_=mstage[:]), r=[("mstage", fb) for fb in range(8)], w=[("xT", tc)])
            S.barrier()
        if stage == 5:
            dump("mergedT", xT[:], [128, 8, T], BF16, key=XTK)
        S.barrier()
        stM.close()

        stW = ExitStack()
        wupt = [stW.enter_context(nc.sbuf_tensor("wupt0", [128, 8, 2 * D], BF16)), None]
        wdnt = [stW.enter_context(nc.sbuf_tensor("wdnt0", [128, 8, D], BF16)), None]

        def load_w(ee):
            sl = ee % 2
            if ee in PC and stage >= 7 and 2 * PC.index(ee) + 1 < pc_state["n"]:
                i = PC.index(ee)
                for hf_ in range(2):
                    S.dma("sp", lambda e, hf_=hf_: e.dma_start(
                        out=wupt[sl][:, :, hf_ * D:(hf_ + 1) * D], in_=wb_d[i, :, hf_ * D:(hf_ + 1) * D].rearrange("(c p) f -> p c f", p=128)),
                        f"wuh{sl}_{hf_}", r=[("pcall", f"pcu{i % 4}")], w=[("wup", sl, hf_)])
                S.dma("sp", lambda e: e.dma_start(out=wdnt[sl][:], in_=wb_d[i, :, 2 * D:3 * D].rearrange("(c p) f -> p c f", p=128)),
                      f"wdh{sl}", r=[("pcall", f"pcd{i % 4}")], w=[("wdn", sl)])
                return
            for hf_ in range(2):
                S.dma("pool", lambda e, ee=ee, sl=sl, hf_=hf_: e.dma_start(
                    out=wupt[sl][:, :, hf_ * D:(hf_ + 1) * D], in_=wup[ee].rearrange("(c p) f -> p c f", p=128)[:, :, hf_ * D:(hf_ + 1) * D]),
                    f"wu{sl}_{hf_}", w=[("wup", sl, hf_)])
            S.dma("pool", lambda e, ee=ee, sl=sl: e.dma_start(out=wdnt[sl][:], in_=wdn[ee].rearrange("(c p) f -> p c f", p=128)),
                  f"wd{sl}", w=[("wdn", sl)])

        YK = [("y", ee) for ee in range(NE)]
        XGK = [("xg", tb, k) for tb in range(16) for k in range(4)]
        if stage >= 6:
          with ExitStack() as st:
            sbl = lambda n, s, dt=F32: st.enter_context(nc.sbuf_tensor(n, list(s), dt))
            wout = sbl("wout", [128, 8, D], BF16)
            lnG = sbl("lnG", [128, 1, D]); lnB = sbl("lnB", [128, 1, D])
            rwf = sbl("rwf", [128, 8, NE]); rbb = sbl("rbb", [128, 1, NE])
            NS = 3
            NB_ = 9
            NQ = 7
            xt = [sbl(f"xt{i}", [128, D]) for i in range(4)]
            r_ = [sbl(f"r_{i}", [128, D]) for i in range(2)]
            h1f = [sbl(f"h1f{i}", [128, D]) for i in range(NS)]
            h1b = [sbl(f"h1b{i}", [128, D], BF16) for i in range(NB_)]
            h1T = [sbl(f"h1T{i}", [128, 8, 128]) for i in range(2)]
            stats = [sbl(f"stats{i}", [128, 2, 6]) for i in range(NS)]
            mv = [sbl(f"mv{i}", [128, 2]) for i in range(NS)]
            rstd = [sbl(f"rstd{i}", [128, 1]) for i in range(NS)]
            mk = lambda n, shp, dt=F32: [sbl(f"{n}{i}", shp, dt) for i in range(NQ)]
            logits = mk("logits", [128, NE]); top8 = mk("top8", [128, 8]); maskf = mk("maskf", [128, NE])
            maskb_all = sbl("maskb_all", [128, 16, NE], BF16)
            negm = mk("negm", [128, 1]); ex = mk("ex", [128, NE]); ssum = mk("ssum", [128, 1]); gfull = mk("gfull", [128, NE])
            val = mk("val", [128, NE]); topv = mk("topv", [128, 8]); oh = [[sbl(f"oh{i}_{k}", [128, NE]) for k in range(4)] for i in range(NQ)]
            S.dma("pool", lambda e: e.dma_start(out=wout[:], in_=w_out.rearrange("(c p) f -> p c f", p=128)), "wout", w=["wout"])
            S.dma("sp", lambda e: e.dma_start(out=lnG[:], in_=ln1g.partition_broadcast(128)), "lnG", w=["l1G"])
            S.dma("sp", lambda e: e.dma_start(out=lnB[:], in_=ln1b.partition_broadcast(128)), "lnB", w=["l1B"])
            S.dma("sp", lambda e: e.dma_start(out=rwf[:], in_=rw.rearrange("(c p) f -> p c f", p=128)), "rwf", w=["rwf"])
            S.dma("sp", lambda e: e.dma_start(out=rbb[:], in_=rb.partition_broadcast(128)), "rbb", w=["rbb"])

            def e2_p(tb):
                x4 = tb % 4
                ts_ = slice(tb * 128, (tb + 1) * 128)
                S.dma("sp", lambda e: e.dma_start(out=xt[x4][:], in_=x[ts_, :]), f"xt{x4}", w=[("xt", x4)])

            def e2_a(tb):
                sl = tb % NS
                x2 = tb % 2
                x4 = tb % 4
                ts_ = slice(tb * 128, (tb + 1) * 128)
                pb = 2 * (tb % 2)
                for hf_ in range(2):
                    S.group("pe", [lambda e, fc=fc, hf_=hf_: e.matmul(
                        PS[pb + hf_][:, :], lhsT=xT[:, fc, ts_], rhs=wout[:, fc, hf_ * 512:(hf_ + 1) * 512], start=(fc == 0), stop=(fc == 7))
                        for fc in range(8)], r=["wout", ("xT", tb // 4)], w=[pk(pb + hf_)])
                    S.op("dve", lambda e, hf_=hf_: e.scalar_tensor_tensor(
                        out=r_[x2][:, hf_ * 512:(hf_ + 1) * 512], in0=xt[x4][:, hf_ * 512:(hf_ + 1) * 512], scalar=ALPHA, in1=PS[pb + hf_][:, :],
                        op0=ALU.mult, op1=ALU.add), r=[("xt", x4), pk(pb + hf_)], w=[("l1r", x2)])
                layer_norm(sl, r_[x2], h1f[sl], lnG[:, 0, :], lnB[:, 0, :], "l1", stats, mv, rstd, rsl=x2)
                S.dma("sp", lambda e: e.dma_start(out=h1_d[ts_, :], in_=h1f[sl][:]), f"h1st{sl}", r=[("l1o", sl)], w=[("h1d", tb)])

            def e2_b1(tb):
                sl = tb % NS
                bs = tb % NB_
                t2 = tb % 2
                S.op("act", lambda e: e.copy(out=h1b[bs][:], in_=h1f[sl][:]), r=[("l1o", sl)], w=[("h1b", bs)])
                for hf_ in range(2):
                    S.group("pe", [lambda e, q=q, hf_=hf_: e.transpose(
                        out=PS[4 + hf_][:, q * 128:(q + 1) * 128], in_=h1f[sl][:, (hf_ * 4 + q) * 128:(hf_ * 4 + q + 1) * 128], identity=identf[:])
                        for q in range(4)], r=[("l1o", sl), "c_identf"], w=[pk(4 + hf_)])
                    if hf_ == 0:
                        S.op("act", lambda e: e.copy(out=h1T[t2][:, 0:4, :], in_=PS[4][:, :].rearrange("p (a b) -> p a b", b=128)),
                             r=[pk(4)], w=[("h1T", t2, 0)])
                    else:
                        S.op("dve", lambda e: e.tensor_copy(out=h1T[t2][:, 4:8, :], in_=PS[5][:, :].rearrange("p (a b) -> p a b", b=128)),
                             r=[pk(5)], w=[("h1T", t2, 1)])

            def e2_b2(tb):
                t2 = tb % 2
                lb_ = 6 + (tb % 2)
                S.group("pe", [lambda e, dc=dc: e.matmul(PS[lb_][:, 0:NE], lhsT=h1T[t2][:, dc, :], rhs=rwf[:, dc, :],
                                                        start=(dc == 0), stop=(dc == 7)) for dc in range(8)],
                        r=[("h1T", t2, 0), ("h1T", t2, 1), "rwf"], w=[pk(lb_)])

            def e2_c(tb):
                q = tb % NQ
                lb_ = 6 + (tb % 2)
                S.op("dve", lambda e: e.tensor_tensor(out=logits[q][:], in0=PS[lb_][:, 0:NE], in1=rbb[:, 0, :], op=ALU.add), r=[pk(lb_), "rbb"], w=[("logits", q)])
                S.op("dve", lambda e: e.max(out=top8[q][:], in_=logits[q][:]), r=[("logits", q)], w=[("top8", q)])

            def e2_d1(tb):
                q = tb % NQ
                S.op("dve", lambda e: e.tensor_scalar(out=maskf[q][:], in0=logits[q][:], scalar1=top8[q][:, 3:4], scalar2=None, op0=ALU.is_ge),
                     r=[("logits", q), ("top8", q)], w=[("maskf", q)])
                S.op("dve", lambda e: e.tensor_scalar(out=negm[q][:], in0=top8[q][:, 0:1], scalar1=-1.0, scalar2=None, op0=ALU.mult),
                     r=[("top8", q)], w=[("negm", q)])
                S.op("dve", lambda e: e.tensor_copy(out=maskb_all[:, tb, :], in_=maskf[q][:]), r=[("maskf", q)], w=[("maskb", tb)])
                S.op("act", lambda e: e.activation(out=ex[q][:], in_=logits[q][:], func=AF.Exp, bias=negm[q][:, 0:1]),
                     r=[("logits", q), ("negm", q)], w=[("ex", q)])

            def e2_d2(tb):
                lb_ = 6 + (tb % 2)
                fns = [lambda e: e.matmul(PS[lb_][:, 64:64 + NE], lhsT=LTb[:, :], rhs=maskb_all[:, tb, :], start=True, stop=(tb == 0), skip_group_check=True)]
                for tp_ in range(tb):
                    fns.append(lambda e, tp_=tp_: e.matmul(PS[lb_][:, 64:64 + NE], lhsT=onesb[:, :], rhs=maskb_all[:, tp_, :], start=False,
                                                         stop=(tp_ == tb - 1), skip_group_check=True))
                S.group("pe", fns, r=[("maskb", t_) for t_ in range(tb + 1)] + ["c_LTb", "c_onesb"], w=[pk(lb_)])

            def e2_e(tb):
                q = tb % NQ
                lb_ = 6 + (tb % 2)
                S.op("dve", lambda e: e.tensor_tensor(out=val[q][:], in0=cvec[:], in1=PS[lb_][:, 64:64 + NE], op=ALU.subtract), r=[pk(lb_)] + CVK, w=[("val", q)])
                S.op("dve", lambda e: e.tensor_tensor(out=ex[q][:], in0=ex[q][:], in1=maskf[q][:], op=ALU.mult), r=[("ex", q), ("maskf", q)], w=[("ex", q)])
                S.op("dve", lambda e: e.tensor_tensor(out=val[q][:], in0=val[q][:], in1=maskf[q][:], op=ALU.mult), r=[("val", q), ("maskf", q)], w=[("val", q)])
                S.op("dve", lambda e: e.reduce_sum(out=ssum[q][:], in_=ex[q][:], axis=AX.X), r=[("ex", q)], w=[("ssum", q)])
                S.op("dve", lambda e: e.max(out=topv[q][:], in_=val[q][:]), r=[("val", q)], w=[("topv", q)])
                S.op("dve", lambda e: e.reciprocal(out=ssum[q][:], in_=ssum[q][:]), r=[("ssum", q)], w=[("ssum", q)])

            def e2_f(tb):
                q = tb % NQ
                for k in range(4):
                    S.op("dve", lambda e, k=k: e.tensor_scalar(out=destk[tb][k][:, :], in0=topv[q][:, k:k + 1], scalar1=-1.0, scalar2=float(NSLOT + 1),
                                                             op0=ALU.mult, op1=ALU.add), r=[("topv", q)], w=[("destk", tb, k)])
                S.op("dve", lambda e: e.tensor_scalar(out=gfull[q][:], in0=ex[q][:], scalar1=ssum[q][:, 0:1], scalar2=None, op0=ALU.mult),
                     r=[("ex", q), ("ssum", q)], w=[("gfull", q)])
                for k in range(4):
                    S.op("dve", lambda e, k=k: e.tensor_scalar(out=oh[q][k][:], in0=val[q][:], scalar1=topv[q][:, k:k + 1], scalar2=None, op0=ALU.is_equal),
                         r=[("val", q), ("topv", q)], w=[("oh", q, k)])

            def e2_g(tb):
                q = tb % NQ
                bs = tb % NB_
                for k in range(4):
                    S.op("dve", lambda e, k=k: e.tensor_tensor(out=oh[q][k][:], in0=oh[q][k][:], in1=gfull[q][:], op=ALU.mult),
                         r=[("oh", q, k), ("gfull", q)], w=[("oh", q, k)])
                for k in range(4):
                    S.dma("pool", lambda e, k=k: e.indirect_dma_start(
                        out=xg_d[:, :], out_offset=bass.IndirectOffsetOnAxis(ap=destk[tb][k][:, :], axis=0), in_=h1b[bs][:, :], in_offset=None,
                        bounds_check=S.regs["bound"], oob_is_err=False), f"sc{bs}", r=[("h1b", bs), ("destk", tb, k)] + XGZ, w=[("xg", tb, k)])
                for k in range(4):
                    S.op("dve", lambda e, k=k: e.reduce_sum(out=gk_all[:, tb, k:k + 1], in_=oh[q][k][:], axis=AX.X), r=[("oh", q, k)], w=[("gk", tb)])

            e2s = [e2_p, (lambda tb: None), e2_a, (lambda tb: None), e2_b1, e2_b2, e2_c, e2_d1, e2_d2, e2_e, e2_f, e2_g]
            for step in range(16 + len(e2s) - 1):
                for k_, f_ in enumerate(e2s):
                    idx = step - k_
                    if 0 <= idx < 16:
                        f_(idx)
                if step == 12 and stage >= 7:
                    load_w(0)
            S.barrier()
        if stage == 6:
            dump("gk", gk_all[:], [128, 16, 4], F32, key=[("gk", tb) for tb in range(16)])
            for tb in range(16):
                for k in range(4):
                    dump(f"destk{tb}_{k}", destk[tb][k][:, :], [128, 1], I32, key=[("destk", tb, k)])
            dump("h1", h1_d, [T, D], F32, key=[("h1d", tb) for tb in range(16)])
            if not os.environ.get("NOXGDUMP"):
                for q in range(12):
                    dump(f"xg{q}", xg_d[q * 1024:(q + 1) * 1024, :], [1024, D], BF16, key=XGK)

        if stage >= 7:
          with ExitStack() as st:
            sbl = lambda n, s, dt=F32: st.enter_context(nc.sbuf_tensor(n, list(s), dt))
            wupt[1] = sbl("wupt1", [128, 8, 2 * D], BF16)
            wdnt[1] = sbl("wdnt1", [128, 8, D], BF16)
            bupT = sbl("bupT", [128, NE * 8, 2])
            bdnb = [sbl(f"bdnb{i}", [128, 1, D]) for i in range(2)]
            xr = [sbl(f"xr{i}", [128, 3, D], BF16) for i in range(2)]
            xgT = [sbl(f"xgT{i}", [128, 8, CAP], BF16) for i in range(2)]
            actT = [sbl(f"actT{i}", [128, 8, CAP], BF16) for i in range(2)]
            ysb = [sbl("ysb0", [128, 3, D])] * 2
            tg = [sbl(f"tg{i}", [128, CAP]) for i in range(2)]
            tsg = [sbl(f"tsg{i}", [128, CAP]) for i in range(2)]
            tl = [sbl(f"tl{i}", [128, CAP]) for i in range(2)]
            S.dma("sp", lambda e: e.dma_start(out=bupT[:], in_=bup.rearrange("e (fb p two) -> p (e fb) two", p=128, two=2)), "bupT", w=["bupT"])
            u = 0
            def load_x(ee):
                sl = ee % 2
                S.dma("sp", lambda e, ee=ee, sl=sl: e.dma_start(out=bdnb[sl][:], in_=bdn[ee:ee + 1, :].partition_broadcast(128)), f"bdnb{sl}", w=[("bdnb", sl)])
                S.dma("sp", lambda e, ee=ee, sl=sl: e.dma_start(out=xr[sl][:], in_=xg_d[ee * CAP:(ee + 1) * CAP, :].rearrange("(s p) d -> p s d", p=128)),
                      f"xr{sl}", r=XGK, w=[("xr", sl)])
            ust = {"u": 0}

            def xpose(ee):
                sl = ee % 2
                for sbk in range(3):
                    bank = ust["u"] % 2
                    ust["u"] += 1
                    S.group("pe", [lambda e, dc=dc, sbk=sbk, bank=bank: e.transpose(
                        out=PSB[bank][:, dc * 128:(dc + 1) * 128], in_=xr[sl][:, sbk, dc * 128:(dc + 1) * 128], identity=identb[:])
                        for dc in range(8)], r=[("xr", sl), "c_identb"], w=[pk(bank)])
                    if sbk % 2 == 0:
                        S.op("act", lambda e, sbk=sbk, bank=bank: e.copy(
                            out=xgT[sl][:, :, sbk * 128:(sbk + 1) * 128], in_=PSB[bank][:, :].rearrange("p (a b) -> p a b", b=128)),
                            r=[pk(bank)], w=[("xgT", sl, sbk)])
                    else:
                        S.op("dve", lambda e, sbk=sbk, bank=bank: e.tensor_copy(
                            out=xgT[sl][:, :, sbk * 128:(sbk + 1) * 128], in_=PSB[bank][:, :].rearrange("p (a b) -> p a b", b=128)),
                            r=[pk(bank)], w=[("xgT", sl, sbk)])

            load_x(0)
            xpose(0)
            for ee in range(NE):
                sl = ee % 2
                if ee + 1 < NE:
                    load_x(ee + 1)
                    load_w(ee + 1)
                def upproj(ee, fb):
                    sl = ee % 2
                    XGT = [("xgT", sl, q) for q in range(3)]
                    fsl = fb % 2
                    gb, lb_ = 2 + 2 * fsl, 3 + 2 * fsl
                    for two, bk in ((0, gb), (1, lb_)):
                        S.group("pe", [lambda e, dc=dc, fb=fb, two=two, bk=bk, sl=sl: e.matmul(
                            PS[bk][:, 0:CAP], lhsT=wupt[sl][:, dc, fb * 256 + two:fb * 256 + 256:2], rhs=xgT[sl][:, dc, :],
                            start=(dc == 0), stop=(dc == 7)) for dc in range(8)], r=[("wup", sl, fb // 4)] + XGT, w=[pk(bk)])
                    bi = ee * 8 + fb
                    S.op("dve", lambda e, gb=gb, fsl=fsl, bi=bi: e.tensor_scalar(
                        out=tg[fsl][:], in0=PS[gb][:, 0:CAP], scalar1=bupT[:, bi, 0:1], scalar2=7.0, op0=ALU.add, op1=ALU.min),
                        r=[pk(gb), "bupT"], w=[("tg", fsl)])
                    S.op("act", lambda e, fsl=fsl: e.activation(out=tsg[fsl][:], in_=tg[fsl][:], func=AF.Sigmoid, scale=1.702),
                         r=[("tg", fsl)], w=[("tsg", fsl)])
                    S.op("dve", lambda e, lb_=lb_, fsl=fsl, bi=bi: e.tensor_scalar(
                        out=tl[fsl][:], in0=PS[lb_][:, 0:CAP], scalar1=bupT[:, bi, 1:2], scalar2=7.0, op0=ALU.add, op1=ALU.min),
                        r=[pk(lb_), "bupT"], w=[("tl", fsl)])
                    S.op("dve", lambda e, fsl=fsl: e.tensor_scalar(out=tl[fsl][:], in0=tl[fsl][:], scalar1=-7.0, scalar2=1.0, op0=ALU.max, op1=ALU.add),
                         r=[("tl", fsl)], w=[("tl", fsl)])
                    S.op("pool", lambda e, fsl=fsl: e.tensor_tensor(out=tg[fsl][:], in0=tg[fsl][:], in1=tsg[fsl][:], op=ALU.mult),
                         r=[("tg", fsl), ("tsg", fsl)], w=[("tg", fsl)])
                    S.op("dve", lambda e, fsl=fsl, fb=fb, sl=sl: e.tensor_tensor(out=actT[sl][:, fb, :], in0=tg[fsl][:], in1=tl[fsl][:], op=ALU.mult),
                         r=[("tg", fsl), ("tl", fsl)], w=[("actT", sl, fb)])

                for fb in range(8):
                    if fb == 0 and ee > 0:
                        continue
                    upproj(ee, fb)
                ACK = [("actT", sl, fb) for fb in range(8)]
                if ee + 1 < NE:
                    xpose(ee + 1)
                    upproj(ee + 1, 0)
                i = 0
                for sbk in range(3):
                    for hf_ in range(2):
                        bank = 6 + i % 2
                        i += 1
                        if i == 1:
                            for fc in range(8):
                                S.op("pe", lambda e, fc=fc, sbk=sbk, hf_=hf_, bank=bank, sl=sl: e.matmul(
                                    PS[bank][:, :], lhsT=actT[sl][:, fc, sbk * 128:(sbk + 1) * 128], rhs=wdnt[sl][:, fc, hf_ * 512:(hf_ + 1) * 512],
                                    start=(fc == 0), stop=(fc == 7)), r=[("wdn", sl), ("actT", sl, fc)], w=[pk(bank)])
                        else:
                            S.group("pe", [lambda e, fc=fc, sbk=sbk, hf_=hf_, bank=bank, sl=sl: e.matmul(
                                PS[bank][:, :], lhsT=actT[sl][:, fc, sbk * 128:(sbk + 1) * 128], rhs=wdnt[sl][:, fc, hf_ * 512:(hf_ + 1) * 512],
                                start=(fc == 0), stop=(fc == 7)) for fc in range(8)], r=[("wdn", sl)] + ACK, w=[pk(bank)])
                        S.op("dve", lambda e, sbk=sbk, hf_=hf_, bank=bank, sl=sl: e.tensor_tensor(
                            out=ysb[sl][:, sbk, hf_ * 512:(hf_ + 1) * 512], in0=PS[bank][:, :], in1=bdnb[sl][:, 0, hf_ * 512:(hf_ + 1) * 512], op=ALU.add),
                            r=[pk(bank), ("bdnb", sl)], w=[("ysb", 0, sbk, hf_)])
                S.dma("sp", lambda e, ee=ee, sl=sl: e.dma_start(out=y_d[ee * CAP:(ee + 1) * CAP, :].rearrange("(s p) d -> p s d", p=128), in_=ysb[sl][:]),
                      "yst0", r=[("ysb", 0, a, b) for a in range(3) for b in range(2)], w=[("y", ee)])
            S.barrier()
        S.barrier()
        stW.close()
        stX.close()

        if stage >= 8:
          with ExitStack() as st:
            sbl = lambda n, s, dt=F32: st.enter_context(nc.sbuf_tensor(n, list(s), dt))
            l2G = sbl("l2G", [128, 1, D]); l2B = sbl("l2B", [128, 1, D])
            NG = 3
            h1r = [sbl(f"h1r{i}", [128, D]) for i in range(NG)]
            yg = [[sbl(f"yg{i}_{k}", [128, D]) for k in range(4)] for i in range(NG)]
            acc = [sbl(f"acc{i}", [128, D]) for i in range(NG)]
            outt = [sbl(f"outt{i}", [128, D]) for i in range(NG)]
            stats = [sbl(f"stats2_{i}", [128, 2, 6]) for i in range(NG)]
            mv = [sbl(f"mv2_{i}", [128, 2]) for i in range(NG)]
            rstd = [sbl(f"rstd2_{i}", [128, 1]) for i in range(NG)]
            S.dma("sp", lambda e: e.dma_start(out=l2G[:], in_=ln2g.partition_broadcast(128)), "l2G", w=["l2G"])
            S.dma("sp", lambda e: e.dma_start(out=l2B[:], in_=ln2b.partition_broadcast(128)), "l2B", w=["l2B"])

            def g_a(tb):
                sl = tb % NG
                ts_ = slice(tb * 128, (tb + 1) * 128)
                S.dma("sp", lambda e: e.dma_start(out=h1r[sl][:], in_=h1_d[ts_, :]), f"h1ld{sl}", r=[("h1d", tb)], w=[("h1r", sl)])
                for k in range(4):
                    S.dma("pool", lambda e, k=k: e.indirect_dma_start(
                        out=yg[sl][k][:, :], out_offset=None, in_=y_d[:, :], in_offset=bass.IndirectOffsetOnAxis(ap=destk[tb][k][:, :], axis=0),
                        bounds_check=S.regs["bound"], oob_is_err=False), f"yg{sl}_{k}", r=YK + [("destk", tb, k)], w=[("yg", sl, k)])

            def g_b(tb):
                sl = tb % NG
                ts_ = slice(tb * 128, (tb + 1) * 128)
                S.op("dve", lambda e: e.tensor_scalar(out=acc[sl][:], in0=h1r[sl][:], scalar1=ALPHA, scalar2=None, op0=ALU.mult),
                     r=[("h1r", sl)], w=[("l2r", sl)])
                for k in range(4):
                    S.op("dve", lambda e, k=k: e.scalar_tensor_tensor(
                        out=acc[sl][:], in0=yg[sl][k][:], scalar=gk_all[:, tb, k:k + 1], in1=acc[sl][:], op0=ALU.mult, op1=ALU.add),
                        r=[("yg", sl, k), ("gk", tb), ("l2r", sl)], w=[("l2r", sl)])
                layer_norm(sl, acc[sl], outt[sl], l2G[:, 0, :], l2B[:, 0, :], "l2", stats, mv, rstd)
                S.dma("sp", lambda e: e.dma_start(out=out[ts_, :], in_=outt[sl][:]), f"out{sl}", r=[("l2o", sl)], w=[("out", tb)])

            for step in range(16 + 2):
                if step < 16:
                    g_a(step)
                if step - 2 >= 0:
                    g_b(step - 2)
            S.barrier()

        S.barrier(full=True)
        with nc.Block() as block:
            S.emit(block)
    return nc, dbg_out


def _run(inputs, stage=99, ncores=8):
    nc, dbg = build_nc(stage=stage)
    names = ["w_in", "hgrn_lb_logits", "hgrn_norm_g", "w_branch_sb", "w_branch_hgrn", "w_out", "ln1_g", "ln1_b",
             "router_w", "router_b", "expert_w_up", "expert_b_up", "expert_w_down", "expert_b_down", "ln2_g", "ln2_b"]
    shared = {}
    for n in names:
        a = np.ascontiguousarray(np.asarray(inputs[n], dtype=np.float32))
        if n in ("w_in", "w_branch_sb", "w_branch_hgrn", "w_out", "router_w", "expert_w_up", "expert_b_up", "expert_w_down", "expert_b_down"):
            a = a[0]
        shared[n] = np.ascontiguousarray(a)
    xs = np.asarray(inputs["x"], dtype=np.float32)
    in_maps = []
    for c in range(ncores):
        m = dict(shared)
        m["x"] = np.ascontiguousarray(xs[c])
        in_maps.append(m)
    res = run_bass_kernel_spmd(nc, in_maps, core_ids=list(range(ncores)))
    return res, dbg


def kernel(**inputs):
    res, _ = _run(inputs)
    return np.stack([np.asarray(r["out"], dtype=np.float32) for r in res.results], axis=0)
```

```python
import os
from contextlib import ExitStack

import numpy as np
import concourse.bass as bass
import concourse.mybir as mybir
from concourse.bass_utils import run_bass_kernel_spmd

F32 = mybir.dt.float32
BF16 = mybir.dt.bfloat16
I32 = mybir.dt.int32
U32 = mybir.dt.uint32
AF = mybir.ActivationFunctionType
ALU = mybir.AluOpType
AX = mybir.AxisListType

T = 2048
D = 1024
NE = 32
CAP = 384
NSLOT = NE * CAP
ALPHA = 2.0 ** 0.25
LN_EPS = 1e-5
RMS_EPS = 1e-6
INW = 5632

ENGS = ("pe", "act", "dve", "pool", "sp")


class Sched:
    def __init__(self, nc, stack):
        self.nc = nc
        self.stack = stack
        self.q = {n: [] for n in ENGS}
        self.sems = {}
        self.cnt = {}
        self.waited = {n: {} for n in ENGS}
        self.lastw = {}
        self.readers = {}
        self.regs = {}
        for n in ENGS:
            self._newsem(n)

    def _newsem(self, name):
        self.sems[name] = self.stack.enter_context(self.nc.semaphore("s_" + name))
        self.cnt[name] = 0

    def _deps(self, eng, r, w):
        need = {}

        def add(d):
            for s, v in d.items():
                if eng == "pe" and s == "pe":
                    continue
                if v > need.get(s, 0):
                    need[s] = v
        for k in r:
            add(self.lastw.get(k, {}))
        for k in w:
            add(self.lastw.get(k, {}))
            add(self.readers.get(k, {}))
        waits = []
        wd = self.waited[eng]
        for s, v in need.items():
            if v > wd.get(s, 0):
                wd[s] = v
                waits.append((s, v))
        return waits

    def _register(self, tok, r, w):
        s, v = tok
        for k in w:
            self.lastw[k] = {s: v}
            self.readers[k] = {}
        for k in r:
            d = self.readers.setdefault(k, {})
            if v > d.get(s, 0):
                d[s] = v

    def op(self, eng, fn, r=(), w=()):
        waits = self._deps(eng, r, w)
        self.cnt[eng] += 1
        tok = (eng, self.cnt[eng])
        self.q[eng].append((waits, fn, (eng, 1)))
        self._register(tok, r, w)

    def group(self, eng, fns, r=(), w=()):
        waits = self._deps(eng, r, w)
        self.cnt[eng] += 1
        tok = (eng, self.cnt[eng])
        for i, fn in enumerate(fns):
            self.q[eng].append((waits if i == 0 else [], fn, (eng, 1) if i == len(fns) - 1 else None))
        self._register(tok, r, w)

    def dma(self, queue, fn, chan, r=(), w=()):
        if chan not in self.sems:
            self._newsem(chan)
        waits = self._deps(queue, r, w)
        self.cnt[chan] += 16
        tok = (chan, self.cnt[chan])
        self.q[queue].append((waits, fn, (chan, 16)))
        self._register(tok, r, w)

    def barrier(self, full=False):
        toks = {s: c for s, c in self.cnt.items() if c > 0 and (full or not s.startswith("pc"))}
        for eng in ENGS:
            wd = self.waited[eng]
            waits = []
            for s, v in toks.items():
                if s == eng:
                    continue
                if v > wd.get(s, 0):
                    wd[s] = v
                    waits.append((s, v))
            if waits:
                self.q[eng].append((waits, None, None))

    def emit(self, block):
        sems = self.sems

        def run(e, lst):
            for waits, fn, inc in lst:
                for s, v in waits:
                    e.wait_ge(sems[s], v)
                if fn is not None:
                    ins = fn(e)
                    if inc is not None:
                        ins.then_inc(sems[inc[0]], inc[1])

        block.tensor(lambda e: run(e, self.q["pe"]))
        block.scalar(lambda e: run(e, self.q["act"]))
        block.vector(lambda e: run(e, self.q["dve"]))
        def run_pool(e):
            self.regs["zero"] = e.to_reg(0.0)
            self.regs["bound"] = e.to_reg(NSLOT - 1)
            run(e, self.q["pool"])
        block.gpsimd(run_pool)
        block.sync(lambda e: run(e, self.q["sp"]))


def build_nc(stage=99, dbg=None):
    nc = bass.Bass("TRN2", target_bir_lowering=False)
    dram = lambda n, s, dt=F32: nc.dram_tensor(n, list(s), dt, kind="ExternalInput").ap()
    x = dram("x", [T, D])
    w_in = dram("w_in", [D, INW])
    lbl = dram("hgrn_lb_logits", [2, 512])
    hng = dram("hgrn_norm_g", [1, 512])
    w_bsb = dram("w_branch_sb", [512, D])
    w_bhg = dram("w_branch_hgrn", [512, D])
    w_out = dram("w_out", [D, D])
    ln1g = dram("ln1_g", [1, D]); ln1b = dram("ln1_b", [1, D])
    rw = dram("router_w", [D, NE]); rb = dram("router_b", [1, NE])
    wup = dram("expert_w_up", [NE, D, 2 * D]); bup = dram("expert_b_up", [NE, 2 * D])
    wdn = dram("expert_w_down", [NE, D, D]); bdn = dram("expert_b_down", [NE, D])
    ln2g = dram("ln2_g", [1, D]); ln2b = dram("ln2_b", [1, D])
    out = nc.dram_tensor("out", [T, D], F32, kind="ExternalOutput").ap()
    xg_d = nc.dram_tensor("xg_d", [NSLOT, D], BF16).ap()
    y_d = nc.dram_tensor("y_d", [NSLOT, D], F32).ap()
    h1_d = nc.dram_tensor("h1_d", [T, D], F32).ap()
    PC = [1, 3, 5, 7, 9, 11, 13, 15, 17, 19, 21, 23, 25]
    wb_d = nc.dram_tensor("wb_d", [len(PC), D, 3 * D], BF16).ap()
    dbg_out = {}

    stack = ExitStack()
    with stack:
        S = Sched(nc, stack)
        sb = lambda n, s, dt=F32: stack.enter_context(nc.sbuf_tensor(n, list(s), dt))
        PS = [stack.enter_context(nc.psum_tensor(f"ps{i}", [128, 512], F32)) for i in range(8)]
        PSB = [p.bitcast(BF16) for p in PS]
        pk = lambda i: ("ps", i)

        def dump(name, ap, shape, dt=F32, key=()):
            o = nc.dram_tensor("dbg_" + name, list(shape), dt, kind="ExternalOutput").ap()
            dbg_out[name] = o
            S.dma("sp", lambda e: e.dma_start(out=o, in_=ap), "dbgch", r=key, w=[("dbg", name)])

        identb = sb("identb", [128, 128], BF16)
        identf = sb("identf", [128, 128], F32)
        negUT = sb("negUT", [128, 128], BF16)
        negOnes = sb("negOnes", [128, 128], BF16)
        zerosb = sb("zerosb", [128, 128], BF16)
        LTb = sb("LTb", [128, 128], BF16)
        onesb = sb("onesb", [128, 128], BF16)
        onesDiv = sb("onesDiv", [128, 128], BF16)
        epsln = sb("epsln", [128, 1], F32)
        epsrms = sb("epsrms", [128, 1], F32)
        cvec = sb("cvec", [128, NE], F32)
        lbraw = sb("lbraw", [128, 2, 4], F32)
        lbd = sb("lbd", [128, 4], F32)
        lbT = sb("lbT", [128, 4], F32)
        omlT = sb("omlT", [128, 4], F32)
        nomlT = sb("nomlT", [128, 4], F32)
        gcol = sb("gcol", [128, 4], F32)
        CK = ["consts"]

        def consts(e):
            pass
        S.op("pool", lambda e: e.memset(identb[:], 1.0), w=["c_identb"])
        S.op("pool", lambda e: e.affine_select(out=identb[:], in_=identb[:], pattern=[[1, 128]], compare_op=ALU.is_equal,
                                               fill=S.regs["zero"], base=0, channel_multiplier=-1), r=["c_identb"], w=["c_identb"])
        S.op("pool", lambda e: e.memset(identf[:], 1.0), w=["c_identf"])
        S.op("pool", lambda e: e.affine_select(out=identf[:], in_=identf[:], pattern=[[1, 128]], compare_op=ALU.is_equal,
                                               fill=S.regs["zero"], base=0, channel_multiplier=-1), r=["c_identf"], w=["c_identf"])
        S.op("pool", lambda e: e.memset(negUT[:], -1.0), w=["c_negUT"])
        S.op("pool", lambda e: e.affine_select(out=negUT[:], in_=negUT[:], pattern=[[-1, 128]], compare_op=ALU.is_ge,
                                               fill=S.regs["zero"], base=0, channel_multiplier=1), r=["c_negUT"], w=["c_negUT"])
        S.op("pool", lambda e: e.memset(negOnes[:], -1.0), w=["c_negOnes"])
        S.op("pool", lambda e: e.memset(zerosb[:], 0.0), w=["c_zerosb"])
        S.op("pool", lambda e: e.memset(onesb[:], 1.0), w=["c_onesb"])
        S.op("pool", lambda e: e.memset(onesDiv[:], 1.0 / 128.0), w=["c_onesDiv"])
        S.op("pool", lambda e: e.memset(LTb[:], 1.0), w=["c_LTb"])
        S.op("pool", lambda e: e.affine_select(out=LTb[:], in_=LTb[:], pattern=[[1, 128]], compare_op=ALU.is_gt,
                                               fill=S.regs["zero"], base=0, channel_multiplier=-1), r=["c_LTb"], w=["c_LTb"])
        S.op("pool", lambda e: e.memset(epsln[:], LN_EPS), w=["c_eps"])
        S.op("pool", lambda e: e.memset(epsrms[:], RMS_EPS), w=["c_eps2"])
        for ee in range(NE):
            S.op("pool", lambda e, ee=ee: e.memset(cvec[:, ee:ee + 1], float(NSLOT + 1 - ee * CAP)), w=[("c_cvec", ee)])
        CVK = [("c_cvec", ee) for ee in range(NE)]
        S.dma("sp", lambda e: e.dma_start(out=lbraw[:], in_=lbl.rearrange("r (h p) -> p r h", p=128), allow_slow_non_contiguous=True), "cst0", w=["lbraw"])
        S.dma("sp", lambda e: e.dma_start(out=gcol[:], in_=hng.rearrange("o (h p) -> p (o h)", p=128), allow_slow_non_contiguous=True), "cst1", w=["gcol"])
        S.op("dve", lambda e: e.tensor_tensor(out=lbd[:], in0=lbraw[:, 0, :], in1=lbraw[:, 1, :], op=ALU.subtract), r=["lbraw"], w=["lbd"])
        S.op("act", lambda e: e.activation(out=lbT[:], in_=lbd[:], func=AF.Sigmoid), r=["lbd"], w=["lbT"])
        S.op("dve", lambda e: e.tensor_scalar(out=omlT[:], in0=lbT[:], scalar1=-1.0, scalar2=1.0, op0=ALU.mult, op1=ALU.add), r=["lbT"], w=["omlT"])
        S.op("dve", lambda e: e.tensor_scalar(out=nomlT[:], in0=lbT[:], scalar1=1.0, scalar2=-1.0, op0=ALU.mult, op1=ALU.add), r=["lbT"], w=["nomlT"])

        gk_all = sb("gk_all", [128, 16, 4], F32)
        destk = [[sb(f"dk{tb}_{k}", [128, 1], I32) for k in range(4)] for tb in range(16)]
        stX = ExitStack()
        xT = stX.enter_context(nc.sbuf_tensor("xT", [128, 8, T], BF16))
        stM = ExitStack()
        o_sbT = stM.enter_context(nc.sbuf_tensor("o_sbT", [64, 8, T], BF16))
        o_hgT = stM.enter_context(nc.sbuf_tensor("o_hgT", [128, 4, T], BF16))
        w_in_v = w_in.rearrange("(c p) f -> p c f", p=128)

        XGZ = [("xgz", ee) for ee in range(NE)]
        pc_state = {"n": 0}

        def precast_next():
            n = pc_state["n"]
            if n >= 2 * len(PC) or stage < 7 or pc_state.get("closed"):
                return
            pc_state["n"] = n + 1
            i, which = n // 2, n % 2
            ee = PC[i]
            if which == 0:
                S.dma("pool", lambda e: e.dma_start(out=wb_d[i, :, 0:2 * D], in_=wup[ee]), f"pcu{i % 4}", w=[("pcu", i)])
            else:
                S.dma("pool", lambda e: e.dma_start(out=wb_d[i, :, 2 * D:3 * D], in_=wdn[ee]), f"pcd{i % 4}", w=[("pcd", i)])

        with ExitStack() as st:
            sbl = lambda n, s, dt=F32: st.enter_context(nc.sbuf_tensor(n, list(s), dt))
            xb = sbl("xb", [128, 16, D], BF16)
            xv = x.rearrange("(n p) d -> p n d", p=128)
            for g in range(4):
                S.dma("pool", lambda e, g=g: e.dma_start(out=xb[:, 4 * g:4 * g + 4, :], in_=xv[:, 4 * g:4 * g + 4, :]),
                      f"xb{g}", w=[("xb", g)])
            i = 0
            for tg in range(4):
                for dc in range(8):
                    bank = i % 2
                    S.group("pe", [lambda e, j=j, tg=tg, dc=dc, bank=bank: e.transpose(
                        out=PSB[bank][:, j * 128:(j + 1) * 128], in_=xb[:, tg * 4 + j, dc * 128:(dc + 1) * 128], identity=identb[:])
                        for j in range(4)], r=[("xb", tg), "c_identb"], w=[pk(bank)])
                    eng = "act" if i % 2 == 0 else "dve"
                    if eng == "act":
                        S.op("act", lambda e, tg=tg, dc=dc, bank=bank: e.copy(out=xT[:, dc, tg * 512:(tg + 1) * 512], in_=PSB[bank][:, 0:512]),
                             r=[pk(bank)], w=[("xT", tg)])
                    else:
                        S.op("dve", lambda e, tg=tg, dc=dc, bank=bank: e.tensor_copy(out=xT[:, dc, tg * 512:(tg + 1) * 512], in_=PSB[bank][:, 0:512]),
                             r=[pk(bank)], w=[("xT", tg)])
                    i += 1
            S.barrier()
        XTK = [("xT", tg) for tg in range(4)]
        if stage == 1:
            dump("xT", xT[:], [128, 8, T], BF16, key=XTK)

        if stage >= 2:
          with ExitStack() as st:
            sbl = lambda n, s, dt=F32: st.enter_context(nc.sbuf_tensor(n, list(s), dt))
            zrow = sbl("zrow", [128, 3, D], BF16)
            S.op("pool", lambda e: e.memset(zrow[:], 0.0), w=["zrow"])
            for ee in range(NE):
                S.dma("sp", lambda e, ee=ee: e.dma_start(out=xg_d[ee * CAP:(ee + 1) * CAP, :].rearrange("(s p) d -> p s d", p=128), in_=zrow[:]),
                      "xgz", r=["zrow"], w=[("xgz", ee)])
            QTs = sbl("QTs", [128, 4, T], BF16)
            KT = sbl("KT", [128, 4, T], BF16)
            V = sbl("V", [128, 16, 512], BF16)
            wg = [sbl(f"wg{i}", [128, 8, 512], BF16) for i in range(2)]
            for g in range(3):
                sl = g % 2
                S.dma("pool", lambda e, g=g, sl=sl: e.dma_start(out=wg[sl][:], in_=w_in_v[:, :, g * 512:(g + 1) * 512]),
                      f"wg{sl}", w=[("wg", sl)])
                if g < 2:
                    dst = QTs if g == 0 else KT
                    i = 0
                    for fb in range(4):
                        for tc in range(4):
                            bank = 2 + (i % 2)
                            S.group("pe", [lambda e, dc=dc, fb=fb, tc=tc, bank=bank, sl=sl: e.matmul(
                                PS[bank][:, :], lhsT=wg[sl][:, dc, fb * 128:(fb + 1) * 128], rhs=xT[:, dc, tc * 512:(tc + 1) * 512],
                                start=(dc == 0), stop=(dc == 7)) for dc in range(8)], r=[("wg", sl), ("xT", tc)], w=[pk(bank)])
                            sc = 0.125 if g == 0 else 1.0
                            if i % 2 == 0:
                                S.op("act", lambda e, dst=dst, fb=fb, tc=tc, bank=bank, sc=sc: e.activation(
                                    out=dst[:, fb, tc * 512:(tc + 1) * 512], in_=PS[bank][:, :], func=AF.Copy, scale=sc),
                                    r=[pk(bank)], w=[("qk", g, fb)])
                            else:
                                S.op("dve", lambda e, dst=dst, fb=fb, tc=tc, bank=bank, sc=sc: e.tensor_scalar(
                                    out=dst[:, fb, tc * 512:(tc + 1) * 512], in0=PS[bank][:, :], scalar1=sc, scalar2=None, op0=ALU.mult),
                                    r=[pk(bank)], w=[("qk", g, fb)])
                            i += 1
                else:
                    for tb in range(16):
                        bank = 2 + (tb % 2)
                        S.group("pe", [lambda e, dc=dc, tb=tb, bank=bank, sl=sl: e.matmul(
                            PS[bank][:, :], lhsT=xT[:, dc, tb * 128:(tb + 1) * 128], rhs=wg[sl][:, dc, :],
                            start=(dc == 0), stop=(dc == 7)) for dc in range(8)], r=[("wg", sl), ("xT", tb // 4)], w=[pk(bank)])
                        if tb % 2 == 0:
                            S.op("act", lambda e, tb=tb, bank=bank: e.copy(out=V[:, tb, :], in_=PS[bank][:, :]), r=[pk(bank)], w=[("V", tb)])
                        else:
                            S.op("dve", lambda e, tb=tb, bank=bank: e.tensor_copy(out=V[:, tb, :], in_=PS[bank][:, :]), r=[pk(bank)], w=[("V", tb)])
            if stage == 2:
                dump("QTs", QTs[:], [128, 4, T], BF16, key=[("qk", 0, fb) for fb in range(4)])
                dump("V", V[:], [128, 16, 512], BF16, key=[("V", tb) for tb in range(16)])

            if stage >= 3:
                Lsum32 = sbl("Lsum32", [128, T], F32)
                Lsum16 = sbl("Lsum16", [128, T], BF16)
                NB = 5
                Eb = [sbl(f"Eb{i}", [128, 512], F32) for i in range(NB)]
                SPb = [sbl(f"SPb{i}", [128, 512], BF16) for i in range(NB)]
                WTb = [sbl(f"WTb{i}", [128, 512], BF16) for i in range(NB)]
                units = []
                for h in range(8):
                    hu = []
                    for kb in range(15, -1, -1):
                        kt0 = kb * 128
                        j0 = kt0 // 512
                        for j in range(j0, 4):
                            t0 = max(kt0, j * 512)
                            hu.append(dict(h=h, kb=kb, j=j, t0=t0, w=(j + 1) * 512 - t0, first=(j == j0), off=t0 - j * 512,
                                           kt0=kt0, hfirst=False, hlast=False))
                    hu[0]["hfirst"] = True
                    hu[-1]["hlast"] = True
                    units += hu
                for i, un in enumerate(units):
                    un["bank"] = i % 4
                    un["sl"] = i % NB

                def st_a(un):
                    h, w, t0, kt0, bank, sl = un["h"], un["w"], un["t0"], un["kt0"], un["bank"], un["sl"]
                    pr, p0 = h // 2, (h % 2) * 64
                    zt = PS[bank]
                    S.op("pe", lambda e: e.matmul(zt[:, 0:w], lhsT=KT[p0:p0 + 64, pr, kt0:kt0 + 128], rhs=QTs[p0:p0 + 64, pr, t0:t0 + w],
                                                  start=True, stop=True), r=[("qk", 0, pr), ("qk", 1, pr)], w=[pk(bank)])
                    S.op("act", lambda e: e.activation(out=Eb[sl][:, 0:w], in_=zt[:, 0:w], func=AF.Exp), r=[pk(bank)], w=[("E", sl)])

                def st_b(un):
                    w, sl = un["w"], un["sl"]
                    S.op("act", lambda e: e.activation(out=SPb[sl][:, 0:w], in_=Eb[sl][:, 0:w], func=AF.Ln, bias=1.0),
                         r=[("E", sl)], w=[("SP", sl)])
                    if un["first"]:
                        S.op("pool", lambda e: e.affine_select(
                            out=SPb[sl][:, 0:128], in_=SPb[sl][:, 0:128], pattern=[[1, 128]], compare_op=ALU.is_gt,
                            fill=S.regs["zero"], base=0, channel_multiplier=-1), r=[("SP", sl)], w=[("SP", sl)])

                def st_c(un):
                    h, kb, j, w, t0, off, bank, sl = un["h"], un["kb"], un["j"], un["w"], un["t0"], un["off"], un["bank"], un["sl"]
                    zt = PS[bank]
                    if un["hfirst"]:
                        S.op("pool", lambda e: e.memset(Lsum32[:], 0.0), w=[("L32", jj) for jj in range(4)])
                        S.op("pool", lambda e: e.memset(Lsum16[:], 0.0), w=[("L16", jj) for jj in range(4)])
                    fns = [lambda e: e.matmul(zt[:, 0:w], lhsT=negUT[:, :], rhs=SPb[sl][:, 0:w], start=False, stop=(kb == 15), skip_group_check=True)]
                    rd = [("SP", sl), "c_negUT"]
                    if kb < 15:
                        fns.append(lambda e: e.matmul(zt[:, 0:w], lhsT=negOnes[:, :], rhs=Lsum16[:, t0:t0 + w], start=False, stop=True, skip_group_check=True))
                        rd += [("L16", j), "c_negOnes"]
                    S.group("pe", fns, r=rd, w=[pk(bank)])
                    S.op("act", lambda e: e.activation(out=WTb[sl][:, 0:w], in_=zt[:, 0:w], func=AF.Exp), r=[pk(bank)], w=[("WT", sl)])
                    if un["first"]:
                        S.op("pool", lambda e: e.affine_select(
                            out=WTb[sl][:, 0:128], in_=WTb[sl][:, 0:128], pattern=[[1, 128]], compare_op=ALU.is_gt,
                            fill=S.regs["zero"], base=0, channel_multiplier=-1), r=[("WT", sl)], w=[("WT", sl)])
                    if kb > 0:
                        S.op("dve", lambda e: e.tensor_tensor(out=Lsum32[:, t0:t0 + w], in0=Lsum32[:, t0:t0 + w], in1=SPb[sl][:, 0:w], op=ALU.add),
                             r=[("SP", sl), ("L32", j)], w=[("L32", j)])
                        S.op("dve", lambda e: e.tensor_copy(out=Lsum16[:, t0:t0 + w], in_=Lsum32[:, t0:t0 + w]), r=[("L32", j)], w=[("L16", j)])

                def st_d(un):
                    h, kb, j, w, t0, off, bank, sl = un["h"], un["kb"], un["j"], un["w"], un["t0"], un["off"], un["bank"], un["sl"]
                    if un["hfirst"]:
                        for jj in range(4):
                            S.op("pe", lambda e, jj=jj: e.matmul(PS[4 + jj][0:64, :], lhsT=zerosb[:, 0:64], rhs=Lsum16[:, jj * 512:(jj + 1) * 512],
                                                               start=True, stop=False, skip_group_check=True),
                                 r=["c_zerosb", ("L16", jj)], w=[pk(4 + jj)])
                    S.op("pe", lambda e: e.matmul(PS[4 + j][0:64, off:off + w], lhsT=V[:, kb, h * 64:(h + 1) * 64], rhs=WTb[sl][:, 0:w],
                                                  start=False, stop=(kb == 0), skip_group_check=True), r=[("WT", sl), ("V", kb)], w=[pk(4 + j)])
                    if un["hlast"]:
                        for jj in range(4):
                            if jj % 2 == 0:
                                S.op("act", lambda e, jj=jj: e.copy(out=o_sbT[:, h, jj * 512:(jj + 1) * 512], in_=PS[4 + jj][0:64, :]),
                                     r=[pk(4 + jj)], w=[("o_sbT", h)])
                            else:
                                S.op("dve", lambda e, jj=jj: e.tensor_copy(out=o_sbT[:, h, jj * 512:(jj + 1) * 512], in_=PS[4 + jj][0:64, :]),
                                     r=[pk(4 + jj)], w=[("o_sbT", h)])

                stages_ = [st_a, st_b, st_c, st_d]
                n = len(units)
                for step in range(n + len(stages_) - 1):
                    for k_, f_ in enumerate(stages_):
                        idx = step - k_
                        if 0 <= idx < n:
                            f_(units[idx])
                    if step % 44 in (4, 32) and step < n - 20:
                        precast_next()
            S.barrier()
        OSK = [("o_sbT", h) for h in range(8)]
        if stage == 3:
            dump("o_sbT", o_sbT[:], [64, 8, T], BF16, key=OSK)

        if stage >= 4:
          with ExitStack() as st:
            sbl = lambda n, s, dt=F32: st.enter_context(nc.sbuf_tensor(n, list(s), dt))
            maskLE8 = sbl("maskLE8", [64, 8, 64], F32)
            rmask = sbl("rmask", [128, 32, 64], F32)
            S.op("pool", lambda e: e.memset(maskLE8[:], 1.0), w=["c_maskLE8"])
            S.op("pool", lambda e: e.affine_select(out=maskLE8[:], in_=maskLE8[:], pattern=[[0, 8], [1, 64]], compare_op=ALU.is_ge,
                                                   fill=S.regs["zero"], base=0, channel_multiplier=-1), r=["c_maskLE8"], w=["c_maskLE8"])
            S.op("pool", lambda e: e.memset(rmask[:], 1.0), w=["c_rmask"])
            S.op("pool", lambda e: e.affine_select(out=rmask[:], in_=rmask[:], pattern=[[0, 32], [1, 64]], compare_op=ALU.is_gt,
                                                   fill=S.regs["zero"], base=0, channel_multiplier=0), r=["c_rmask"], w=["c_rmask"])
            wh = [sbl(f"wh{i}", [128, 8, 4, 128], BF16) for i in range(2)]
            sgq = [sbl(f"sgq{i}", [128, 512], F32) for i in range(2)]
            qT = sbl("qT", [128, T], F32)
            kT = sbl("kT", [128, T], F32)
            fT = sbl("fT", [128, T], F32)
            gT = fT
            bT = sbl("bT", [128, T], F32)
            ebt = sbl("ebt", [128, T], F32)
            sgT = sbl("sgT", [128, T], BF16)
            qeT = sbl("qeT", [128, T], BF16)
            keT = sbl("keT", [128, T], BF16)
            klT = sbl("klT", [128, T], BF16)
            elast = sbl("elast", [128, 32], F32)
            vtok = sbl("vtok", [64, 32, 128], BF16)
            kltok = sbl("kltok", [64, 32, 128], BF16)
            scT = [sbl(f"scT{i}", [64, 8, 64], BF16) for i in range(2)]
            S32 = [sbl(f"S32_{i}", [128, 128], F32) for i in range(2)]
            S16 = [sbl(f"S16_{i}", [128, 128], BF16) for i in range(4)]
            oT = qT
            osq = klT
            rs = [sbl(f"rs{i}", [128, 512], F32) for i in range(2)]
            onr = [sbl(f"onr{i}", [128, 512], F32) for i in range(2)]
            def load_wh(hh):
                sl = hh % 2
                for kk in range(4):
                    c0 = 1536 + kk * 512 + hh * 128
                    S.dma("pool", lambda e, sl=sl, kk=kk, c0=c0: e.dma_start(out=wh[sl][:, :, kk, :], in_=w_in_v[:, :, c0:c0 + 128]),
                          f"wh{sl}_{kk}", w=[("wh", sl, kk)])
            load_wh(0)
            for hh in range(4):
                sl = hh % 2
                if hh + 1 < 4:
                    load_wh(hh + 1)
                precast_next()
                precast_next()
                i = 0
                for kk in (0, 1, 3):
                    for tc in range(4):
                        bank = i % 2
                        cs = slice(tc * 512, (tc + 1) * 512)
                        S.group("pe", [lambda e, dc=dc, kk=kk, tc=tc, bank=bank, sl=sl: e.matmul(
                            PS[bank][:, :], lhsT=wh[sl][:, dc, kk, :], rhs=xT[:, dc, tc * 512:(tc + 1) * 512],
                            start=(dc == 0), stop=(dc == 7)) for dc in range(8)], r=[("wh", sl, kk), ("xT", tc)], w=[pk(bank)])
                        sq_ = sgq[i % 2]
                        if kk == 0:
                            S.op("act", lambda e, bank=bank, sq_=sq_: e.activation(out=sq_[:], in_=PS[bank][:, :], func=AF.Sigmoid),
                                 r=[pk(bank)], w=[("sgq", i % 2)])
                            S.op("dve", lambda e, bank=bank, sq_=sq_, cs=cs: e.tensor_tensor(out=qT[:, cs], in0=PS[bank][:, :], in1=sq_[:], op=ALU.mult),
                                 r=[pk(bank), ("sgq", i % 2)], w=["qT"])
                        elif kk == 1:
                            S.op("act", lambda e, bank=bank, sq_=sq_: e.activation(out=sq_[:], in_=PS[bank][:, :], func=AF.Sigmoid),
                                 r=[pk(bank)], w=[("sgq", i % 2)])
                            S.op("dve", lambda e, sq_=sq_, cs=cs, hh=hh: e.tensor_scalar(
                                out=fT[:, cs], in0=sq_[:], scalar1=omlT[:, hh:hh + 1], scalar2=lbT[:, hh:hh + 1], op0=ALU.mult, op1=ALU.add),
                                r=[("sgq", i % 2), "omlT", "lbT"], w=["fT"])
                            S.op("dve", lambda e, sq_=sq_, cs=cs, hh=hh: e.tensor_scalar(
                                out=kT[:, cs], in0=sq_[:], scalar1=nomlT[:, hh:hh + 1], scalar2=omlT[:, hh:hh + 1], op0=ALU.mult, op1=ALU.add),
                                r=[("sgq", i % 2), "omlT", "nomlT"], w=["kT"])
                        else:
                            S.op("act", lambda e, bank=bank, cs=cs: e.activation(out=sgT[:, cs], in_=PS[bank][:, :], func=AF.Sigmoid),
                                 r=[pk(bank)], w=["sgT"])
                        i += 1
                for g4 in range(8):
                    bank = 2 + g4 % 2
                    fns = []
                    for jj in range(4):
                        c = g4 * 4 + jj
                        for dc in range(8):
                            fns.append(lambda e, dc=dc, c=c, jj=jj, bank=bank, sl=sl: e.matmul(
                                PS[bank][0:64, jj * 128:(jj + 1) * 128], lhsT=xT[:, dc, c * 64:(c + 1) * 64], rhs=wh[sl][:, dc, 2, :],
                                start=(dc == 0), stop=(dc == 7), skip_group_check=True))
                    S.group("pe", fns, r=[("wh", sl, 2)] + XTK, w=[pk(bank)])
                    S.op("act", lambda e, g4=g4, bank=bank: e.copy(out=vtok[:, g4 * 4:(g4 + 1) * 4, :], in_=PS[bank][0:64, :].rearrange("p (a b) -> p a b", b=128)),
                         r=[pk(bank)], w=["vtok"])
                S.op("act", lambda e: e.activation(out=gT[:], in_=fT[:], func=AF.Ln), r=["fT"], w=["fT"])
                S.op("dve", lambda e: e.tensor_tensor_scan(out=bT[:], data0=rmask[:].rearrange("p a b -> p (a b)"), data1=gT[:], initial=0.0,
                                                           op0=ALU.mult, op1=ALU.add), r=["fT", "c_rmask"], w=["bT"])
                S.op("act", lambda e: e.activation(out=elast[:], in_=bT[:, 63::64], func=AF.Exp), r=["bT"], w=["elast"])
                S.op("act", lambda e: e.activation(out=ebt[:], in_=bT[:], func=AF.Exp), r=["bT"], w=["ebt"])
                S.op("dve", lambda e: e.tensor_tensor(out=qeT[:], in0=qT[:], in1=ebt[:], op=ALU.mult), r=["qT", "ebt"], w=["qeT"])
                S.op("act", lambda e: e.activation(out=ebt[:], in_=bT[:], func=AF.Exp, scale=-1.0), r=["bT"], w=["ebt"])
                S.op("dve", lambda e: e.tensor_tensor(out=keT[:], in0=kT[:], in1=ebt[:], op=ALU.mult), r=["kT", "ebt"], w=["keT"])
                S.op("dve", lambda e: e.tensor_tensor(out=ebt[:], in0=kT[:], in1=ebt[:], op=ALU.mult), r=["kT", "ebt"], w=["ebt"])
                S.op("dve", lambda e: e.tensor_tensor(out=klT[:].rearrange("p (c k) -> p c k", k=64), in0=ebt[:].rearrange("p (c k) -> p c k", k=64),
                                                      in1=elast[:, :].unsqueeze(2).broadcast_to([128, 32, 64]), op=ALU.mult),
                     r=["ebt", "elast"], w=["klT"])
                for g8 in range(4):
                    bank = 2 + g8 % 2
                    S.group("pe", [lambda e, jj=jj, g8=g8, bank=bank: e.transpose(
                        out=PSB[bank][0:64, jj * 128:(jj + 1) * 128], in_=klT[:, (g8 * 8 + jj) * 64:(g8 * 8 + jj + 1) * 64], identity=identb[:])
                        for jj in range(8)], r=["klT", "c_identb"], w=[pk(bank)])
                    S.op("dve", lambda e, g8=g8, bank=bank: e.tensor_copy(
                        out=kltok[:, g8 * 8:(g8 + 1) * 8, :], in_=PSB[bank][0:64, :].rearrange("p (a b) -> p a b", b=128)),
                        r=[pk(bank)], w=["kltok"])
                for g8 in range(4):
                    if g8 == 2:
                        precast_next()
                    scb = 4 + g8 % 2
                    ssl = g8 % 2
                    S.group("pe", [lambda e, jj=jj, g8=g8, scb=scb: e.matmul(
                        PS[scb][0:64, jj * 64:(jj + 1) * 64], lhsT=keT[:, (g8 * 8 + jj) * 64:(g8 * 8 + jj + 1) * 64],
                        rhs=qeT[:, (g8 * 8 + jj) * 64:(g8 * 8 + jj + 1) * 64], start=True, stop=True, skip_group_check=True)
                        for jj in range(8)], r=["keT", "qeT"], w=[pk(scb)])
                    S.op("dve", lambda e, scb=scb, ssl=ssl: e.tensor_tensor(
                        out=scT[ssl][:], in0=PS[scb][0:64, :].rearrange("p (a b) -> p a b", b=64), in1=maskLE8[:], op=ALU.mult),
                        r=[pk(scb), "c_maskLE8"], w=[("scT", ssl)])
                    ob = 6 + g8 % 2
                    for jj in range(8):
                        c = g8 * 8 + jj
                        if jj % 4 == 0:
                            db = 2 + (c // 4) % 2
                            S.group("pe", [lambda e, q=q, c=c, db=db: e.matmul(
                                PS[db][:, q * 128:(q + 1) * 128], lhsT=kltok[:, c + q, :], rhs=vtok[:, c + q, :],
                                start=True, stop=True, skip_group_check=True) for q in range(4)], r=["kltok", "vtok"], w=[pk(db)])
                        db = 2 + (c // 4) % 2
                        cur = c % 4
                        fns = []
                        rd = [("scT", ssl), "vtok", "qeT"]
                        if c > 0:
                            fns.append(lambda e, ob=ob, jj=jj, c=c, cur=cur: e.matmul(
                                PS[ob][:, jj * 64:(jj + 1) * 64], lhsT=S16[cur][:, :], rhs=qeT[:, c * 64:(c + 1) * 64],
                                start=True, stop=False, skip_group_check=True))
                            rd.append(("S16", cur))
                        fns.append(lambda e, ob=ob, jj=jj, c=c, ssl=ssl: e.matmul(
                            PS[ob][:, jj * 64:(jj + 1) * 64], lhsT=vtok[:, c, :], rhs=scT[ssl][:, jj, :],
                            start=(c == 0), stop=True, skip_group_check=True))
                        S.group("pe", fns, r=rd, w=[pk(ob)])
                        nxt = (c + 1) % 4
                        so, sn = c % 2, (c + 1) % 2
                        dsl = PS[db][:, (c % 4) * 128:(c % 4 + 1) * 128]
                        if c == 0:
                            S.op("dve", lambda e, dsl=dsl, sn=sn: e.tensor_copy(out=S32[sn][:], in_=dsl), r=[pk(db)], w=[("S32", sn)])
                            S.op("act", lambda e, nxt=nxt, sn=sn: e.copy(out=S16[nxt][:], in_=S32[sn][:]), r=[("S32", sn)], w=[("S16", nxt)])
                        elif c < 31:
                            S.op("dve", lambda e, dsl=dsl, c=c, so=so, sn=sn: e.scalar_tensor_tensor(
                                out=S32[sn][:], in0=S32[so][:], scalar=elast[:, c:c + 1], in1=dsl, op0=ALU.mult, op1=ALU.add),
                                r=[pk(db), ("S32", so), "elast"], w=[("S32", sn)])
                            S.op("act", lambda e, nxt=nxt, sn=sn: e.copy(out=S16[nxt][:], in_=S32[sn][:]), r=[("S32", sn)], w=[("S16", nxt)])
                    S.op("act", lambda e, ob=ob, g8=g8: e.copy(out=oT[:, g8 * 512:(g8 + 1) * 512], in_=PS[ob][:, :]), r=[pk(ob)], w=["qT"])
                S.op("act", lambda e: e.activation(out=osq[:], in_=oT[:], func=AF.Square), r=["qT"], w=["klT"])
                for tc in range(4):
                    bank = tc % 2
                    cs = slice(tc * 512, (tc + 1) * 512)
                    S.op("pe", lambda e, bank=bank, cs=cs: e.matmul(PS[bank][:, :], lhsT=onesDiv[:, :], rhs=osq[:, cs], start=True, stop=True),
                         r=["klT", "c_onesDiv"], w=[pk(bank)])
                    S.op("act", lambda e, bank=bank, tc=tc: e.activation(out=rs[tc % 2][:], in_=PS[bank][:, :], func=AF.Sqrt, bias=epsrms[:, 0:1]),
                         r=[pk(bank), "c_eps2"], w=[("rs", tc % 2)])
                    S.op("dve", lambda e, tc=tc: e.reciprocal(out=rs[tc % 2][:], in_=rs[tc % 2][:]), r=[("rs", tc % 2)], w=[("rs", tc % 2)])
                    S.op("dve", lambda e, tc=tc, cs=cs, hh=hh: e.scalar_tensor_tensor(
                        out=onr[tc % 2][:], in0=oT[:, cs], scalar=gcol[:, hh:hh + 1], in1=rs[tc % 2][:], op0=ALU.mult, op1=ALU.mult),
                        r=["qT", ("rs", tc % 2), "gcol"], w=[("onr", tc % 2)])
                    S.op("dve", lambda e, tc=tc, cs=cs, hh=hh: e.tensor_tensor(out=o_hgT[:, hh, cs], in0=onr[tc % 2][:], in1=sgT[:, cs], op=ALU.mult),
                         r=[("onr", tc % 2), "sgT"], w=[("o_hgT", hh)])
            S.barrier()
        OHK = [("o_hgT", hh) for hh in range(4)]
        for c_ in list(S.cnt):
            if c_.startswith("pc") and S.cnt[c_] > 0:
                S.lastw[("pcall", c_)] = {c_: S.cnt[c_]}
        pc_state["closed"] = True
        if stage == 4:
            dump("o_hgT", o_hgT[:], [128, 4, T], BF16, key=OHK)


        def layer_norm(sl, src, dst, Gt, Bt, tag, stats, mv, rstd, rsl=None):
            rsl = sl if rsl is None else rsl
            for hf_ in range(2):
                S.op("dve", lambda e, hf_=hf_: e.bn_stats(out=stats[sl][:, hf_, :], in_=src[:, hf_ * 512:(hf_ + 1) * 512]),
                     r=[(tag + "r", rsl)], w=[(tag + "st", sl, hf_)])
            S.op("dve", lambda e: e.bn_aggr(out=mv[sl][:], in_=stats[sl][:].rearrange("p a b -> p (a b)")),
                 r=[(tag + "st", sl, 0), (tag + "st", sl, 1)], w=[(tag + "mv", sl)])
            S.op("act", lambda e: e.activation(out=rstd[sl][:], in_=mv[sl][:, 1:2], func=AF.Sqrt, bias=epsln[:, 0:1]),
                 r=[(tag + "mv", sl), "c_eps"], w=[(tag + "rstd", sl)])
            S.op("dve", lambda e: e.reciprocal(out=rstd[sl][:], in_=rstd[sl][:]), r=[(tag + "rstd", sl)], w=[(tag + "rstd", sl)])
            S.op("dve", lambda e: e.tensor_scalar(out=src[:], in0=src[:], scalar1=mv[sl][:, 0:1], scalar2=rstd[sl][:, 0:1],
                                                  op0=ALU.subtract, op1=ALU.mult),
                 r=[(tag + "r", rsl), (tag + "mv", sl), (tag + "rstd", sl)], w=[(tag + "r", rsl)])
            S.op("pool", lambda e: e.tensor_tensor(out=dst[:], in0=src[:], in1=Gt[:], op=ALU.mult),
                 r=[(tag + "r", rsl), tag + "G"], w=[(tag + "o", sl)])
            S.op("pool", lambda e: e.tensor_tensor(out=dst[:], in0=dst[:], in1=Bt[:], op=ALU.add),
                 r=[(tag + "o", sl), tag + "B"], w=[(tag + "o", sl)])

        if stage >= 5:
          with ExitStack() as st:
            sbl = lambda n, s, dt=F32: st.enter_context(nc.sbuf_tensor(n, list(s), dt))
            wgate = sbl("wgate", [128, 8, 2, D], BF16)
            wbsb = sbl("wbsb", [64, 8, D], BF16)
            wbhg = sbl("wbhg", [128, 4, D], BF16)
            mstage = sbl("mstage", [128, 8, 512], BF16)
            sg1 = [sbl(f"sg1_{i}", [128, 512], F32) for i in range(2)]
            sg2 = [sbl(f"sg2_{i}", [128, 512], F32) for i in range(2)]
            S.dma("pool", lambda e: e.dma_start(out=wbsb[:], in_=w_bsb.rearrange("(h p) f -> p h f", p=64)), "wbsb", w=["wbsb"])
            S.dma("pool", lambda e: e.dma_start(out=wbhg[:], in_=w_bhg.rearrange("(h p) f -> p h f", p=128)), "wbhg", w=["wbhg"])
            for q4 in range(4):
                for kk in range(2):
                    c0 = 3584 + kk * 1024 + q4 * 256
                    S.dma("pool", lambda e, kk=kk, c0=c0, q4=q4: e.dma_start(out=wgate[:, :, kk, q4 * 256:(q4 + 1) * 256], in_=w_in_v[:, :, c0:c0 + 256]),
                          f"wgate{q4}_{kk}", w=[("wgate", q4, kk)])
            i = 0
            for tc in range(4):
                cs = slice(tc * 512, (tc + 1) * 512)
                for fb in range(8):
                    sl = i % 2
                    b0 = 4 * (i % 2)
                    for kk in range(2):
                        S.group("pe", [lambda e, dc=dc, kk=kk, fb=fb, b0=b0, cs=cs: e.matmul(
                            PS[b0 + kk][:, :], lhsT=wgate[:, dc, kk, fb * 128:(fb + 1) * 128], rhs=xT[:, dc, cs], start=(dc == 0), stop=(dc == 7))
                            for dc in range(8)], r=[("wgate", fb // 2, kk), ("xT", tc)], w=[pk(b0 + kk)])
                    S.group("pe", [lambda e, h=h, fb=fb, b0=b0, cs=cs: e.matmul(
                        PS[b0 + 2][:, :], lhsT=wbsb[:, h, fb * 128:(fb + 1) * 128], rhs=o_sbT[:, h, cs], start=(h == 0), stop=(h == 7))
                        for h in range(8)], r=["wbsb"] + OSK, w=[pk(b0 + 2)])
                    S.group("pe", [lambda e, hh=hh, fb=fb, b0=b0, cs=cs: e.matmul(
                        PS[b0 + 3][:, :], lhsT=wbhg[:, hh, fb * 128:(fb + 1) * 128], rhs=o_hgT[:, hh, cs], start=(hh == 0), stop=(hh == 3))
                        for hh in range(4)], r=["wbhg"] + OHK, w=[pk(b0 + 3)])
                    S.op("act", lambda e, sl=sl, b0=b0: e.activation(out=sg1[sl][:], in_=PS[b0][:, :], func=AF.Sigmoid), r=[pk(b0)], w=[("sg1", sl)])
                    S.op("act", lambda e, sl=sl, b0=b0: e.activation(out=sg2[sl][:], in_=PS[b0 + 1][:, :], func=AF.Sigmoid), r=[pk(b0 + 1)], w=[("sg2", sl)])
                    S.op("dve", lambda e, sl=sl, b0=b0: e.tensor_tensor(out=sg1[sl][:], in0=sg1[sl][:], in1=PS[b0 + 2][:, :], op=ALU.mult),
                         r=[("sg1", sl), pk(b0 + 2)], w=[("sg1", sl)])
                    S.op("dve", lambda e, sl=sl, b0=b0: e.tensor_tensor(out=sg2[sl][:], in0=sg2[sl][:], in1=PS[b0 + 3][:, :], op=ALU.mult),
                         r=[("sg2", sl), pk(b0 + 3)], w=[("sg2", sl)])
                    S.op("pool", lambda e, sl=sl, fb=fb: e.tensor_tensor(out=mstage[:, fb, :], in0=sg1[sl][:], in1=sg2[sl][:], op=ALU.add),
                         r=[("sg1", sl), ("sg2", sl)], w=[("mstage", fb)])
                    i += 1
                S.op("act", lambda e, cs=cs: e.copy(out=xT[:, :, cs], in_=mstage[:]), r=[("mstage", fb) for fb in range(8)], w=[("xT", tc)])
            S.barrier()
        if stage == 5:
            dump("mergedT", xT[:], [128, 8, T], BF16, key=XTK)
        S.barrier()
        stM.close()

        stW = ExitStack()
        wupt = [stW.enter_context(nc.sbuf_tensor("wupt0", [128, 8, 2 * D], BF16)), None]
        wdnt = [stW.enter_context(nc.sbuf_tensor("wdnt0", [128, 8, D], BF16)), None]

        def load_w(ee):
            sl = ee % 2
            if ee in PC and stage >= 7 and 2 * PC.index(ee) + 1 < pc_state["n"]:
                i = PC.index(ee)
                for hf_ in range(2):
                    S.dma("sp", lambda e, hf_=hf_: e.dma_start(
                        out=wupt[sl][:, :, hf_ * D:(hf_ + 1) * D], in_=wb_d[i, :, hf_ * D:(hf_ + 1) * D].rearrange("(c p) f -> p c f", p=128)),
                        f"wuh{sl}_{hf_}", r=[("pcall", f"pcu{i % 4}")], w=[("wup", sl, hf_)])
                S.dma("sp", lambda e: e.dma_start(out=wdnt[sl][:], in_=wb_d[i, :, 2 * D:3 * D].rearrange("(c p) f -> p c f", p=128)),
                      f"wdh{sl}", r=[("pcall", f"pcd{i % 4}")], w=[("wdn", sl)])
                return
            for hf_ in range(2):
                S.dma("pool", lambda e, ee=ee, sl=sl, hf_=hf_: e.dma_start(
                    out=wupt[sl][:, :, hf_ * D:(hf_ + 1) * D], in_=wup[ee].rearrange("(c p) f -> p c f", p=128)[:, :, hf_ * D:(hf_ + 1) * D]),
                    f"wu{sl}_{hf_}", w=[("wup", sl, hf_)])
            S.dma("pool", lambda e, ee=ee, sl=sl: e.dma_start(out=wdnt[sl][:], in_=wdn[ee].rearrange("(c p) f -> p c f", p=128)),
                  f"wd{sl}", w=[("wdn", sl)])

        YK = [("y", ee) for ee in range(NE)]
        XGK = [("xg", tb, k) for tb in range(16) for k in range(4)]
        if stage >= 6:
          with ExitStack() as st:
            sbl = lambda n, s, dt=F32: st.enter_context(nc.sbuf_tensor(n, list(s), dt))
            wout = sbl("wout", [128, 8, D], BF16)
            lnG = sbl("lnG", [128, 1, D]); lnB = sbl("lnB", [128, 1, D])
            rwf = sbl("rwf", [128, 8, NE]); rbb = sbl("rbb", [128, 1, NE])
            NS = 3
            NB_ = 9
            NQ = 7
            xt = [sbl(f"xt{i}", [128, D]) for i in range(4)]
            r_ = [sbl(f"r_{i}", [128, D]) for i in range(2)]
            h1f = [sbl(f"h1f{i}", [128, D]) for i in range(NS)]
            h1b = [sbl(f"h1b{i}", [128, D], BF16) for i in range(NB_)]
            h1T = [sbl(f"h1T{i}", [128, 8, 128]) for i in range(2)]
            stats = [sbl(f"stats{i}", [128, 2, 6]) for i in range(NS)]
            mv = [sbl(f"mv{i}", [128, 2]) for i in range(NS)]
            rstd = [sbl(f"rstd{i}", [128, 1]) for i in range(NS)]
            mk = lambda n, shp, dt=F32: [sbl(f"{n}{i}", shp, dt) for i in range(NQ)]
            logits = mk("logits", [128, NE]); top8 = mk("top8", [128, 8]); maskf = mk("maskf", [128, NE])
            maskb_all = sbl("maskb_all", [128, 16, NE], BF16)
            negm = mk("negm", [128, 1]); ex = mk("ex", [128, NE]); ssum = mk("ssum", [128, 1]); gfull = mk("gfull", [128, NE])
            val = mk("val", [128, NE]); topv = mk("topv", [128, 8]); oh = [[sbl(f"oh{i}_{k}", [128, NE]) for k in range(4)] for i in range(NQ)]
            S.dma("pool", lambda e: e.dma_start(out=wout[:], in_=w_out.rearrange("(c p) f -> p c f", p=128)), "wout", w=["wout"])
            S.dma("sp", lambda e: e.dma_start(out=lnG[:], in_=ln1g.partition_broadcast(128)), "lnG", w=["l1G"])
            S.dma("sp", lambda e: e.dma_start(out=lnB[:], in_=ln1b.partition_broadcast(128)), "lnB", w=["l1B"])
            S.dma("sp", lambda e: e.dma_start(out=rwf[:], in_=rw.rearrange("(c p) f -> p c f", p=128)), "rwf", w=["rwf"])
            S.dma("sp", lambda e: e.dma_start(out=rbb[:], in_=rb.partition_broadcast(128)), "rbb", w=["rbb"])

            def e2_p(tb):
                x4 = tb % 4
                ts_ = slice(tb * 128, (tb + 1) * 128)
                S.dma("sp", lambda e: e.dma_start(out=xt[x4][:], in_=x[ts_, :]), f"xt{x4}", w=[("xt", x4)])

            def e2_a(tb):
                sl = tb % NS
                x2 = tb % 2
                x4 = tb % 4
                ts_ = slice(tb * 128, (tb + 1) * 128)
                pb = 2 * (tb % 2)
                for hf_ in range(2):
                    S.group("pe", [lambda e, fc=fc, hf_=hf_: e.matmul(
                        PS[pb + hf_][:, :], lhsT=xT[:, fc, ts_], rhs=wout[:, fc, hf_ * 512:(hf_ + 1) * 512], start=(fc == 0), stop=(fc == 7))
                        for fc in range(8)], r=["wout", ("xT", tb // 4)], w=[pk(pb + hf_)])
                    S.op("dve", lambda e, hf_=hf_: e.scalar_tensor_tensor(
                        out=r_[x2][:, hf_ * 512:(hf_ + 1) * 512], in0=xt[x4][:, hf_ * 512:(hf_ + 1) * 512], scalar=ALPHA, in1=PS[pb + hf_][:, :],
                        op0=ALU.mult, op1=ALU.add), r=[("xt", x4), pk(pb + hf_)], w=[("l1r", x2)])
                layer_norm(sl, r_[x2], h1f[sl], lnG[:, 0, :], lnB[:, 0, :], "l1", stats, mv, rstd, rsl=x2)
                S.dma("sp", lambda e: e.dma_start(out=h1_d[ts_, :], in_=h1f[sl][:]), f"h1st{sl}", r=[("l1o", sl)], w=[("h1d", tb)])

            def e2_b1(tb):
                sl = tb % NS
                bs = tb % NB_
                t2 = tb % 2
                S.op("act", lambda e: e.copy(out=h1b[bs][:], in_=h1f[sl][:]), r=[("l1o", sl)], w=[("h1b", bs)])
                for hf_ in range(2):
                    S.group("pe", [lambda e, q=q, hf_=hf_: e.transpose(
                        out=PS[4 + hf_][:, q * 128:(q + 1) * 128], in_=h1f[sl][:, (hf_ * 4 + q) * 128:(hf_ * 4 + q + 1) * 128], identity=identf[:])
                        for q in range(4)], r=[("l1o", sl), "c_identf"], w=[pk(4 + hf_)])
                    if hf_ == 0:
                        S.op("act", lambda e: e.copy(out=h1T[t2][:, 0:4, :], in_=PS[4][:, :].rearrange("p (a b) -> p a b", b=128)),
                             r=[pk(4)], w=[("h1T", t2, 0)])
                    else:
                        S.op("dve", lambda e: e.tensor_copy(out=h1T[t2][:, 4:8, :], in_=PS[5][:, :].rearrange("p (a b) -> p a b", b=128)),
                             r=[pk(5)], w=[("h1T", t2, 1)])

            def e2_b2(tb):
                t2 = tb % 2
                lb_ = 6 + (tb % 2)
                S.group("pe", [lambda e, dc=dc: e.matmul(PS[lb_][:, 0:NE], lhsT=h1T[t2][:, dc, :], rhs=rwf[:, dc, :],
                                                        start=(dc == 0), stop=(dc == 7)) for dc in range(8)],
                        r=[("h1T", t2, 0), ("h1T", t2, 1), "rwf"], w=[pk(lb_)])

            def e2_c(tb):
                q = tb % NQ
                lb_ = 6 + (tb % 2)
                S.op("dve", lambda e: e.tensor_tensor(out=logits[q][:], in0=PS[lb_][:, 0:NE], in1=rbb[:, 0, :], op=ALU.add), r=[pk(lb_), "rbb"], w=[("logits", q)])
                S.op("dve", lambda e: e.max(out=top8[q][:], in_=logits[q][:]), r=[("logits", q)], w=[("top8", q)])

            def e2_d1(tb):
                q = tb % NQ
                S.op("dve", lambda e: e.tensor_scalar(out=maskf[q][:], in0=logits[q][:], scalar1=top8[q][:, 3:4], scalar2=None, op0=ALU.is_ge),
                     r=[("logits", q), ("top8", q)], w=[("maskf", q)])
                S.op("dve", lambda e: e.tensor_scalar(out=negm[q][:], in0=top8[q][:, 0:1], scalar1=-1.0, scalar2=None, op0=ALU.mult),
                     r=[("top8", q)], w=[("negm", q)])
                S.op("dve", lambda e: e.tensor_copy(out=maskb_all[:, tb, :], in_=maskf[q][:]), r=[("maskf", q)], w=[("maskb", tb)])
                S.op("act", lambda e: e.activation(out=ex[q][:], in_=logits[q][:], func=AF.Exp, bias=negm[q][:, 0:1]),
                     r=[("logits", q), ("negm", q)], w=[("ex", q)])

            def e2_d2(tb):
                lb_ = 6 + (tb % 2)
                fns = [lambda e: e.matmul(PS[lb_][:, 64:64 + NE], lhsT=LTb[:, :], rhs=maskb_all[:, tb, :], start=True, stop=(tb == 0), skip_group_check=True)]
                for tp_ in range(tb):
                    fns.append(lambda e, tp_=tp_: e.matmul(PS[lb_][:, 64:64 + NE], lhsT=onesb[:, :], rhs=maskb_all[:, tp_, :], start=False,
                                                         stop=(tp_ == tb - 1), skip_group_check=True))
                S.group("pe", fns, r=[("maskb", t_) for t_ in range(tb + 1)] + ["c_LTb", "c_onesb"], w=[pk(lb_)])

            def e2_e(tb):
                q = tb % NQ
                lb_ = 6 + (tb % 2)
                S.op("dve", lambda e: e.tensor_tensor(out=val[q][:], in0=cvec[:], in1=PS[lb_][:, 64:64 + NE], op=ALU.subtract), r=[pk(lb_)] + CVK, w=[("val", q)])
                S.op("dve", lambda e: e.tensor_tensor(out=ex[q][:], in0=ex[q][:], in1=maskf[q][:], op=ALU.mult), r=[("ex", q), ("maskf", q)], w=[("ex", q)])
                S.op("dve", lambda e: e.tensor_tensor(out=val[q][:], in0=val[q][:], in1=maskf[q][:], op=ALU.mult), r=[("val", q), ("maskf", q)], w=[("val", q)])
                S.op("dve", lambda e: e.reduce_sum(out=ssum[q][:], in_=ex[q][:], axis=AX.X), r=[("ex", q)], w=[("ssum", q)])
                S.op("dve", lambda e: e.max(out=topv[q][:], in_=val[q][:]), r=[("val", q)], w=[("topv", q)])
                S.op("dve", lambda e: e.reciprocal(out=ssum[q][:], in_=ssum[q][:]), r=[("ssum", q)], w=[("ssum", q)])

            def e2_f(tb):
                q = tb % NQ
                for k in range(4):
                    S.op("dve", lambda e, k=k: e.tensor_scalar(out=destk[tb][k][:, :], in0=topv[q][:, k:k + 1], scalar1=-1.0, scalar2=float(NSLOT + 1),
                                                             op0=ALU.mult, op1=ALU.add), r=[("topv", q)], w=[("destk", tb, k)])
                S.op("dve", lambda e: e.tensor_scalar(out=gfull[q][:], in0=ex[q][:], scalar1=ssum[q][:, 0:1], scalar2=None, op0=ALU.mult),
                     r=[("ex", q), ("ssum", q)], w=[("gfull", q)])
                for k in range(4):
                    S.op("dve", lambda e, k=k: e.tensor_scalar(out=oh[q][k][:], in0=val[q][:], scalar1=topv[q][:, k:k + 1], scalar2=None, op0=ALU.is_equal),
                         r=[("val", q), ("topv", q)], w=[("oh", q, k)])

            def e2_g(tb):
                q = tb % NQ
                bs = tb % NB_
                for k in range(4):
                    S.op("dve", lambda e, k=k: e.tensor_tensor(out=oh[q][k][:], in0=oh[q][k][:], in1=gfull[q][:], op=ALU.mult),
                         r=[("oh", q, k), ("gfull", q)], w=[("oh", q, k)])
                for k in range(4):
                    S.dma("pool", lambda e, k=k: e.indirect_dma_start(
                        out=xg_d[:, :], out_offset=bass.IndirectOffsetOnAxis(ap=destk[tb][k][:, :], axis=0), in_=h1b[bs][:, :], in_offset=None,
                        bounds_check=S.regs["bound"], oob_is_err=False), f"sc{bs}", r=[("h1b", bs), ("destk", tb, k)] + XGZ, w=[("xg", tb, k)])
                for k in range(4):
                    S.op("dve", lambda e, k=k: e.reduce_sum(out=gk_all[:, tb, k:k + 1], in_=oh[q][k][:], axis=AX.X), r=[("oh", q, k)], w=[("gk", tb)])

            e2s = [e2_p, (lambda tb: None), e2_a, (lambda tb: None), e2_b1, e2_b2, e2_c, e2_d1, e2_d2, e2_e, e2_f, e2_g]
            for step in range(16 + len(e2s) - 1):
                for k_, f_ in enumerate(e2s):
                    idx = step - k_
                    if 0 <= idx < 16:
                        f_(idx)
                if step == 12 and stage >= 7:
                    load_w(0)
            S.barrier()
        if stage == 6:
            dump("gk", gk_all[:], [128, 16, 4], F32, key=[("gk", tb) for tb in range(16)])
            for tb in range(16):
                for k in range(4):
                    dump(f"destk{tb}_{k}", destk[tb][k][:, :], [128, 1], I32, key=[("destk", tb, k)])
            dump("h1", h1_d, [T, D], F32, key=[("h1d", tb) for tb in range(16)])
            if not os.environ.get("NOXGDUMP"):
                for q in range(12):
                    dump(f"xg{q}", xg_d[q * 1024:(q + 1) * 1024, :], [1024, D], BF16, key=XGK)

        if stage >= 7:
          with ExitStack() as st:
            sbl = lambda n, s, dt=F32: st.enter_context(nc.sbuf_tensor(n, list(s), dt))
            wupt[1] = sbl("wupt1", [128, 8, 2 * D], BF16)
            wdnt[1] = sbl("wdnt1", [128, 8, D], BF16)
            bupT = sbl("bupT", [128, NE * 8, 2])
            bdnb = [sbl(f"bdnb{i}", [128, 1, D]) for i in range(2)]
            xr = [sbl(f"xr{i}", [128, 3, D], BF16) for i in range(2)]
            xgT = [sbl(f"xgT{i}", [128, 8, CAP], BF16) for i in range(2)]
            actT = [sbl(f"actT{i}", [128, 8, CAP], BF16) for i in range(2)]
            ysb = [sbl("ysb0", [128, 3, D])] * 2
            tg = [sbl(f"tg{i}", [128, CAP]) for i in range(2)]
            tsg = [sbl(f"tsg{i}", [128, CAP]) for i in range(2)]
            tl = [sbl(f"tl{i}", [128, CAP]) for i in range(2)]
            S.dma("sp", lambda e: e.dma_start(out=bupT[:], in_=bup.rearrange("e (fb p two) -> p (e fb) two", p=128, two=2)), "bupT", w=["bupT"])
            u = 0
            def load_x(ee):
                sl = ee % 2
                S.dma("sp", lambda e, ee=ee, sl=sl: e.dma_start(out=bdnb[sl][:], in_=bdn[ee:ee + 1, :].partition_broadcast(128)), f"bdnb{sl}", w=[("bdnb", sl)])
                S.dma("sp", lambda e, ee=ee, sl=sl: e.dma_start(out=xr[sl][:], in_=xg_d[ee * CAP:(ee + 1) * CAP, :].rearrange("(s p) d -> p s d", p=128)),
                      f"xr{sl}", r=XGK, w=[("xr", sl)])
            ust = {"u": 0}

            def xpose(ee):
                sl = ee % 2
                for sbk in range(3):
                    bank = ust["u"] % 2
                    ust["u"] += 1
                    S.group("pe", [lambda e, dc=dc, sbk=sbk, bank=bank: e.transpose(
                        out=PSB[bank][:, dc * 128:(dc + 1) * 128], in_=xr[sl][:, sbk, dc * 128:(dc + 1) * 128], identity=identb[:])
                        for dc in range(8)], r=[("xr", sl), "c_identb"], w=[pk(bank)])
                    if sbk % 2 == 0:
                        S.op("act", lambda e, sbk=sbk, bank=bank: e.copy(
                            out=xgT[sl][:, :, sbk * 128:(sbk + 1) * 128], in_=PSB[bank][:, :].rearrange("p (a b) -> p a b", b=128)),
                            r=[pk(bank)], w=[("xgT", sl, sbk)])
                    else:
                        S.op("dve", lambda e, sbk=sbk, bank=bank: e.tensor_copy(
                            out=xgT[sl][:, :, sbk * 128:(sbk + 1) * 128], in_=PSB[bank][:, :].rearrange("p (a b) -> p a b", b=128)),
                            r=[pk(bank)], w=[("xgT", sl, sbk)])

            load_x(0)
            xpose(0)
            for ee in range(NE):
                sl = ee % 2
                if ee + 1 < NE:
                    load_x(ee + 1)
                    load_w(ee + 1)
                XGT = [("xgT", sl, q) for q in range(3)]
                for fb in range(8):
                    fsl = fb % 2
                    gb, lb_ = 2 + 2 * fsl, 3 + 2 * fsl
                    for two, bk in ((0, gb), (1, lb_)):
                        S.group("pe", [lambda e, dc=dc, fb=fb, two=two, bk=bk, sl=sl: e.matmul(
                            PS[bk][:, 0:CAP], lhsT=wupt[sl][:, dc, fb * 256 + two:fb * 256 + 256:2], rhs=xgT[sl][:, dc, :],
                            start=(dc == 0), stop=(dc == 7)) for dc in range(8)], r=[("wup", sl, fb // 4)] + XGT, w=[pk(bk)])
                    bi = ee * 8 + fb
                    S.op("dve", lambda e, gb=gb, fsl=fsl, bi=bi: e.tensor_scalar(
                        out=tg[fsl][:], in0=PS[gb][:, 0:CAP], scalar1=bupT[:, bi, 0:1], scalar2=7.0, op0=ALU.add, op1=ALU.min),
                        r=[pk(gb), "bupT"], w=[("tg", fsl)])
                    S.op("act", lambda e, fsl=fsl: e.activation(out=tsg[fsl][:], in_=tg[fsl][:], func=AF.Sigmoid, scale=1.702),
                         r=[("tg", fsl)], w=[("tsg", fsl)])
                    S.op("dve", lambda e, lb_=lb_, fsl=fsl, bi=bi: e.tensor_scalar(
                        out=tl[fsl][:], in0=PS[lb_][:, 0:CAP], scalar1=bupT[:, bi, 1:2], scalar2=7.0, op0=ALU.add, op1=ALU.min),
                        r=[pk(lb_), "bupT"], w=[("tl", fsl)])
                    S.op("dve", lambda e, fsl=fsl: e.tensor_scalar(out=tl[fsl][:], in0=tl[fsl][:], scalar1=-7.0, scalar2=1.0, op0=ALU.max, op1=ALU.add),
                         r=[("tl", fsl)], w=[("tl", fsl)])
                    S.op("pool", lambda e, fsl=fsl: e.tensor_tensor(out=tg[fsl][:], in0=tg[fsl][:], in1=tsg[fsl][:], op=ALU.mult),
                         r=[("tg", fsl), ("tsg", fsl)], w=[("tg", fsl)])
                    S.op("dve", lambda e, fsl=fsl, fb=fb, sl=sl: e.tensor_tensor(out=actT[sl][:, fb, :], in0=tg[fsl][:], in1=tl[fsl][:], op=ALU.mult),
                         r=[("tg", fsl), ("tl", fsl)], w=[("actT", sl, fb)])
                ACK = [("actT", sl, fb) for fb in range(8)]
                if ee + 1 < NE:
                    xpose(ee + 1)
                i = 0
                for sbk in range(3):
                    for hf_ in range(2):
                        bank = 6 + i % 2
                        i += 1
                        if i == 1:
                            for fc in range(8):
                                S.op("pe", lambda e, fc=fc, sbk=sbk, hf_=hf_, bank=bank, sl=sl: e.matmul(
                                    PS[bank][:, :], lhsT=actT[sl][:, fc, sbk * 128:(sbk + 1) * 128], rhs=wdnt[sl][:, fc, hf_ * 512:(hf_ + 1) * 512],
                                    start=(fc == 0), stop=(fc == 7)), r=[("wdn", sl), ("actT", sl, fc)], w=[pk(bank)])
                        else:
                            S.group("pe", [lambda e, fc=fc, sbk=sbk, hf_=hf_, bank=bank, sl=sl: e.matmul(
                                PS[bank][:, :], lhsT=actT[sl][:, fc, sbk * 128:(sbk + 1) * 128], rhs=wdnt[sl][:, fc, hf_ * 512:(hf_ + 1) * 512],
                                start=(fc == 0), stop=(fc == 7)) for fc in range(8)], r=[("wdn", sl)] + ACK, w=[pk(bank)])
                        S.op("dve", lambda e, sbk=sbk, hf_=hf_, bank=bank, sl=sl: e.tensor_tensor(
                            out=ysb[sl][:, sbk, hf_ * 512:(hf_ + 1) * 512], in0=PS[bank][:, :], in1=bdnb[sl][:, 0, hf_ * 512:(hf_ + 1) * 512], op=ALU.add),
                            r=[pk(bank), ("bdnb", sl)], w=[("ysb", 0, sbk, hf_)])
                S.dma("sp", lambda e, ee=ee, sl=sl: e.dma_start(out=y_d[ee * CAP:(ee + 1) * CAP, :].rearrange("(s p) d -> p s d", p=128), in_=ysb[sl][:]),
                      "yst0", r=[("ysb", 0, a, b) for a in range(3) for b in range(2)], w=[("y", ee)])
            S.barrier()
        S.barrier()
        stW.close()
        stX.close()

        if stage >= 8:
          with ExitStack() as st:
            sbl = lambda n, s, dt=F32: st.enter_context(nc.sbuf_tensor(n, list(s), dt))
            l2G = sbl("l2G", [128, 1, D]); l2B = sbl("l2B", [128, 1, D])
            NG = 3
            h1r = [sbl(f"h1r{i}", [128, D]) for i in range(NG)]
            yg = [[sbl(f"yg{i}_{k}", [128, D]) for k in range(4)] for i in range(NG)]
            acc = [sbl(f"acc{i}", [128, D]) for i in range(NG)]
            outt = [sbl(f"outt{i}", [128, D]) for i in range(NG)]
            stats = [sbl(f"stats2_{i}", [128, 2, 6]) for i in range(NG)]
            mv = [sbl(f"mv2_{i}", [128, 2]) for i in range(NG)]
            rstd = [sbl(f"rstd2_{i}", [128, 1]) for i in range(NG)]
            S.dma("sp", lambda e: e.dma_start(out=l2G[:], in_=ln2g.partition_broadcast(128)), "l2G", w=["l2G"])
            S.dma("sp", lambda e: e.dma_start(out=l2B[:], in_=ln2b.partition_broadcast(128)), "l2B", w=["l2B"])

            def g_a(tb):
                sl = tb % NG
                ts_ = slice(tb * 128, (tb + 1) * 128)
                S.dma("sp", lambda e: e.dma_start(out=h1r[sl][:], in_=h1_d[ts_, :]), f"h1ld{sl}", r=[("h1d", tb)], w=[("h1r", sl)])
                for k in range(4):
                    S.dma("pool", lambda e, k=k: e.indirect_dma_start(
                        out=yg[sl][k][:, :], out_offset=None, in_=y_d[:, :], in_offset=bass.IndirectOffsetOnAxis(ap=destk[tb][k][:, :], axis=0),
                        bounds_check=S.regs["bound"], oob_is_err=False), f"yg{sl}_{k}", r=YK + [("destk", tb, k)], w=[("yg", sl, k)])

            def g_b(tb):
                sl = tb % NG
                ts_ = slice(tb * 128, (tb + 1) * 128)
                S.op("dve", lambda e: e.tensor_scalar(out=acc[sl][:], in0=h1r[sl][:], scalar1=ALPHA, scalar2=None, op0=ALU.mult),
                     r=[("h1r", sl)], w=[("l2r", sl)])
                for k in range(4):
                    S.op("dve", lambda e, k=k: e.scalar_tensor_tensor(
                        out=acc[sl][:], in0=yg[sl][k][:], scalar=gk_all[:, tb, k:k + 1], in1=acc[sl][:], op0=ALU.mult, op1=ALU.add),
                        r=[("yg", sl, k), ("gk", tb), ("l2r", sl)], w=[("l2r", sl)])
                layer_norm(sl, acc[sl], outt[sl], l2G[:, 0, :], l2B[:, 0, :], "l2", stats, mv, rstd)
                S.dma("sp", lambda e: e.dma_start(out=out[ts_, :], in_=outt[sl][:]), f"out{sl}", r=[("l2o", sl)], w=[("out", tb)])

            for step in range(16 + 2):
                if step < 16:
                    g_a(step)
                if step - 2 >= 0:
                    g_b(step - 2)
            S.barrier()

        S.barrier(full=True)
        with nc.Block() as block:
            S.emit(block)
    return nc, dbg_out


def _run(inputs, stage=99, ncores=8):
    nc, dbg = build_nc(stage=stage)
    names = ["w_in", "hgrn_lb_logits", "hgrn_norm_g", "w_branch_sb", "w_branch_hgrn", "w_out", "ln1_g", "ln1_b",
             "router_w", "router_b", "expert_w_up", "expert_b_up", "expert_w_down", "expert_b_down", "ln2_g", "ln2_b"]
    shared = {}
    for n in names:
        a = np.ascontiguousarray(np.asarray(inputs[n], dtype=np.float32))
        if n in ("w_in", "w_branch_sb", "w_branch_hgrn", "w_out", "router_w", "expert_w_up", "expert_b_up", "expert_w_down", "expert_b_down"):
            a = a[0]
        shared[n] = np.ascontiguousarray(a)
    xs = np.asarray(inputs["x"], dtype=np.float32)
    in_maps = []
    for c in range(ncores):
        m = dict(shared)
        m["x"] = np.ascontiguousarray(xs[c])
        in_maps.append(m)
    res = run_bass_kernel_spmd(nc, in_maps, core_ids=list(range(ncores)))
    return res, dbg


def kernel(**inputs):
    res, _ = _run(inputs)
    return np.stack([np.asarray(r["out"], dtype=np.float32) for r in res.results], axis=0)
```
